# Optimizing a Trainium2 kernel written in Bass

```python
import math
import jax, jax.numpy as jnp
from jax import lax
import numpy as np

D_MODEL = 1024
BATCH = 8
SEQ = 2048
DEPTH = 4

A_HEADS = 4
A_HEAD_DIM = 128
IDX_HEADS = 8
IDX_DIM = 64
TOPK_MAX = 256
B_HEADS = 4
B_KEY_DIM = 128
B_VAL_DIM = 128
B_CHUNK = 64
C_HEADS = 4
C_Q_RANK = 384
C_KV_RANK = 256
C_NOPE = 128
C_ROPE = 64
C_V = 128
ROPE_THETA = 10000.0
REL_BUCKETS = 32
REL_MAX_DIST = 128
D_FF = -(-8 * D_MODEL // (3 * 256)) * 256
QBLOCK = 128
EPS = 1e-6
N_BRANCH = 3
NEG_BIG = -1e30
LB_FLOOR = 1e-30

A_WIDTH = A_HEADS * A_HEAD_DIM
B_WIDTH = B_HEADS * B_VAL_DIM
C_WIDTH = C_HEADS * C_V
IN_SPLITS = (A_HEADS * A_HEAD_DIM, A_HEAD_DIM, A_HEAD_DIM,
             IDX_HEADS * IDX_DIM, IDX_DIM, IDX_HEADS,
             B_HEADS * B_KEY_DIM, B_HEADS * B_KEY_DIM,
             B_HEADS * B_VAL_DIM, B_HEADS * B_VAL_DIM,
             C_Q_RANK, C_KV_RANK, C_ROPE,
             N_BRANCH * D_MODEL)
IN_COLS = sum(IN_SPLITS)

kernel_name = 'hybrid_dsa_hgrn2_mla_block'


def rms_norm(x, g):
    x32 = x.astype(jnp.float32)
    y = x32 * lax.rsqrt(jnp.mean(x32 * x32, axis=-1, keepdims=True) + EPS)
    return (y * g.astype(jnp.float32)).astype(x.dtype)


def apply_rope(x, cos, sin):
    x32 = x.astype(jnp.float32)
    x1, x2 = jnp.split(x32, 2, axis=-1)
    return jnp.concatenate([x1 * cos - x2 * sin, x2 * cos + x1 * sin], axis=-1).astype(x.dtype)


def t5_bucket(dist):
    max_exact = REL_BUCKETS // 2
    d = jnp.maximum(dist, 0)
    dl = jnp.maximum(d, max_exact).astype(jnp.float32)
    large = max_exact + (jnp.log(dl / max_exact) / math.log(REL_MAX_DIST / max_exact)
                         * (REL_BUCKETS - max_exact)).astype(jnp.int32)
    large = jnp.minimum(large, REL_BUCKETS - 1)
    return jnp.where(d < max_exact, d, large)


def dsa_attention(q, k, v, iq, ik, iw, positions, rel_bias):
    bsz, s_len = q.shape[0], q.shape[1]
    topk = min(TOPK_MAX, s_len // 4)
    n_blk = s_len // QBLOCK
    key_idx = jnp.arange(s_len)
    scale = A_HEAD_DIM ** -0.5
    idx_scale = (IDX_DIM ** -0.5) * (IDX_HEADS ** -0.5)
    ik32 = ik.astype(jnp.float32)
    gather = jax.vmap(lambda arr, ix: arr[ix])

    def block(i):
        start = i * QBLOCK
        qb = lax.dynamic_slice_in_dim(q, start, QBLOCK, axis=1)
        iqb = lax.dynamic_slice_in_dim(iq, start, QBLOCK, axis=1).astype(jnp.float32)
        iwb = lax.dynamic_slice_in_dim(iw, start, QBLOCK, axis=1).astype(jnp.float32)
        pb = lax.dynamic_slice_in_dim(positions, start, QBLOCK, axis=1)
        t_idx = start + jnp.arange(QBLOCK)
        causal = key_idx[None, :] <= t_idx[:, None]
        logits = jnp.einsum('bthd,bsd->bths', iqb, ik32)
        score = jnp.einsum('bths,bth->bts', jax.nn.relu(logits), iwb) * idx_scale
        score = jnp.where(causal[None], score, NEG_BIG)
        _, sel = lax.top_k(score, topk)
        k_sel = gather(k, sel)
        v_sel = gather(v, sel)
        p_sel = gather(positions, sel)
        bias = rel_bias[t5_bucket(pb[:, :, None] - p_sel)]
        s = (jnp.einsum('bthd,btkd->bthk', qb, k_sel).astype(jnp.float32) * scale
             + jnp.moveaxis(bias, -1, 2).astype(jnp.float32))
        valid = sel <= t_idx[None, :, None]
        s = jnp.where(valid[:, :, None, :], s, NEG_BIG)
        p = jax.nn.softmax(s, axis=-1).astype(v.dtype)
        return jnp.einsum('bthk,btkd->bthd', p, v_sel)

    out = lax.map(block, jnp.arange(n_blk))
    return jnp.moveaxis(out, 0, 1).reshape(bsz, s_len, A_WIDTH)


def hgrn2_mixer(q, f_raw, inp, g, lb, out_gain):
    bsz, s_len = q.shape[0], q.shape[1]
    f32 = jnp.float32
    n_chunk = s_len // B_CHUNK
    fr = f_raw.astype(f32)
    log_lb = jnp.log(jnp.maximum(lb, LB_FLOOR))
    log_f = jnp.logaddexp(log_lb, jnp.log1p(-lb) + jax.nn.log_sigmoid(fr))
    k_in = (1.0 - lb) * jax.nn.sigmoid(-fr)
    q_s = q.astype(f32) * (B_KEY_DIM ** -0.5)

    def chunks(t, d):
        return t.astype(f32).reshape(bsz, n_chunk, B_CHUNK, B_HEADS, d).transpose(1, 0, 3, 2, 4)

    causal = jnp.tril(jnp.ones((B_CHUNK, B_CHUNK), dtype=bool))

    def step(state, xs):
        qc, kc, vc, lfc = xs
        b = jnp.cumsum(lfc, axis=2)
        o_inter = jnp.einsum('bhck,bhkv->bhcv', qc * jnp.exp(b), state)
        diff = b[:, :, :, None, :] - b[:, :, None, :, :]
        decay = jnp.exp(jnp.where(causal[None, None, :, :, None], diff, NEG_BIG))
        attn = jnp.einsum('bhtk,bhsk,bhtsk->bhts', qc, kc, decay)
        o_intra = jnp.einsum('bhts,bhsv->bhtv', attn, vc)
        b_last = b[:, :, -1:, :]
        new_state = (jnp.exp(b_last[:, :, 0, :])[..., None] * state
                     + jnp.einsum('bhsk,bhsv->bhkv', kc * jnp.exp(b_last - b), vc))
        return new_state, o_inter + o_intra

    xs = (chunks(q_s, B_KEY_DIM), chunks(k_in, B_KEY_DIM), chunks(inp, B_VAL_DIM), chunks(log_f, B_KEY_DIM))
    state0 = jnp.zeros((bsz, B_HEADS, B_KEY_DIM, B_VAL_DIM), f32)
    _, o = lax.scan(step, state0, xs)
    o = o.transpose(1, 0, 3, 2, 4).reshape(bsz, s_len, B_HEADS, B_VAL_DIM)
    o = rms_norm(o, out_gain) * jax.nn.silu(g.astype(f32).reshape(bsz, s_len, B_HEADS, B_VAL_DIM))
    return o.reshape(bsz, s_len, B_WIDTH).astype(q.dtype)


def mla_attention(q_nope, q_pe, k_nope, k_pe, v):
    bsz, s_len = q_nope.shape[0], q_nope.shape[1]
    n_blk = s_len // QBLOCK
    scale = (C_NOPE + C_ROPE) ** -0.5
    key_idx = jnp.arange(s_len)

    def block(i):
        start = i * QBLOCK
        qn = lax.dynamic_slice_in_dim(q_nope, start, QBLOCK, axis=1)
        qp = lax.dynamic_slice_in_dim(q_pe, start, QBLOCK, axis=1)
        t_idx = start + jnp.arange(QBLOCK)
        causal = key_idx[None, :] <= t_idx[:, None]
        s = (jnp.einsum('bthd,bshd->bhts', qn, k_nope)
             + jnp.einsum('bthr,bsr->bhts', qp, k_pe)).astype(jnp.float32) * scale
        s = jnp.where(causal[None, None], s, NEG_BIG)
        p = jax.nn.softmax(s, axis=-1).astype(v.dtype)
        return jnp.einsum('bhts,bshd->bthd', p, v)

    out = lax.map(block, jnp.arange(n_blk))
    return jnp.moveaxis(out, 0, 1).reshape(bsz, s_len, C_WIDTH)


def setup_inputs(seed: int = 0) -> dict:
    key = jax.random.key(seed)
    ks = jax.random.split(key, 24)
    f32 = jnp.float32
    res_scale = (2 * DEPTH) ** -0.5

    def dense(k, shape, fan_in, scale=1.0):
        return jax.random.normal(k, shape, f32) * (scale * fan_in ** -0.5)

    def gain(k, shape):
        return 1.0 + 0.02 * jax.random.normal(k, shape, f32)

    x = jax.random.normal(ks[0], (BATCH, SEQ, D_MODEL), f32)
    offset = jax.random.randint(ks[1], (BATCH, 1), 0, 4096, dtype=jnp.int32)
    positions = offset + jnp.arange(SEQ, dtype=jnp.int32)[None, :]
    return {
        'x': x,
        'positions': positions,
        'w_in': dense(ks[2], (DEPTH, D_MODEL, IN_COLS), D_MODEL),
        'w_up_a': dense(ks[3], (DEPTH, A_WIDTH, D_MODEL), A_WIDTH),
        'w_up_b': dense(ks[4], (DEPTH, B_WIDTH, D_MODEL), B_WIDTH),
        'w_up_c': dense(ks[5], (DEPTH, C_WIDTH, D_MODEL), C_WIDTH),
        'w_out': dense(ks[6], (DEPTH, D_MODEL, D_MODEL), D_MODEL, res_scale),
        'mla_q_norm': gain(ks[7], (DEPTH, C_Q_RANK)),
        'mla_w_qb': dense(ks[8], (DEPTH, C_Q_RANK, C_HEADS * (C_NOPE + C_ROPE)), C_Q_RANK),
        'mla_kv_norm': gain(ks[9], (DEPTH, C_KV_RANK)),
        'mla_w_kvb': dense(ks[10], (DEPTH, C_KV_RANK, C_HEADS * (C_NOPE + C_V)), C_KV_RANK),
        'hgrn_lb_logits': 0.5 * jax.random.normal(ks[11], (DEPTH, B_HEADS * B_KEY_DIM), f32),
        'hgrn_out_norm': gain(ks[12], (DEPTH, B_VAL_DIM)),
        'rel_bias': 0.5 * jax.random.normal(ks[13], (REL_BUCKETS, A_HEADS), f32),
        'attn_norm': gain(ks[14], (DEPTH, D_MODEL)),
        'ffn_norm': gain(ks[15], (DEPTH, D_MODEL)),
        'w_ffn_gate': dense(ks[16], (DEPTH, D_MODEL, D_FF), D_MODEL),
        'w_ffn_up': dense(ks[17], (DEPTH, D_MODEL, D_FF), D_MODEL),
        'w_ffn_down': dense(ks[18], (DEPTH, D_FF, D_MODEL), D_FF, res_scale),
        'final_norm': gain(ks[19], (D_MODEL,)),
    }


def reference(x, positions, w_in, w_up_a, w_up_b, w_up_c, w_out, mla_q_norm, mla_w_qb, mla_kv_norm,
              mla_w_kvb, hgrn_lb_logits, hgrn_out_norm, rel_bias, attn_norm, ffn_norm, w_ffn_gate,
              w_ffn_up, w_ffn_down, final_norm):
    bsz, s_len, _ = x.shape
    f32 = jnp.float32
    split_pts = np.cumsum(IN_SPLITS)[:-1].tolist()
    p_lb = jax.nn.softmax(hgrn_lb_logits.astype(f32), axis=0)
    lower_bounds = jnp.cumsum(p_lb, axis=0) - p_lb[0:1]
    inv_freq = ROPE_THETA ** (-jnp.arange(0, C_ROPE, 2, dtype=f32) / C_ROPE)
    ang = positions.astype(f32)[..., None] * inv_freq
    cos, sin = jnp.cos(ang), jnp.sin(ang)

    for l in range(DEPTH):
        h = rms_norm(x, attn_norm[l])
        proj = h @ w_in[l]
        (aq, ak, av, iq, ik, iw, bq, bf, bi, bg, cq, ckv, ckpe, gate_logits) = jnp.split(proj, split_pts, axis=-1)
        o_a = dsa_attention(aq.reshape(bsz, s_len, A_HEADS, A_HEAD_DIM), ak, av,
                            iq.reshape(bsz, s_len, IDX_HEADS, IDX_DIM), ik, iw, positions, rel_bias)
        o_b = hgrn2_mixer(bq, bf, bi, bg, lower_bounds[l], hgrn_out_norm[l])
        q_c = (rms_norm(cq, mla_q_norm[l]) @ mla_w_qb[l]).reshape(bsz, s_len, C_HEADS, C_NOPE + C_ROPE)
        q_nope, q_pe = q_c[..., :C_NOPE], q_c[..., C_NOPE:]
        kv_c = (rms_norm(ckv, mla_kv_norm[l]) @ mla_w_kvb[l]).reshape(bsz, s_len, C_HEADS, C_NOPE + C_V)
        k_nope, v_c = kv_c[..., :C_NOPE], kv_c[..., C_NOPE:]
        q_pe = apply_rope(q_pe, cos[:, :, None, :], sin[:, :, None, :])
        k_pe = apply_rope(ckpe, cos, sin)
        o_c = mla_attention(q_nope, q_pe, k_nope, k_pe, v_c)
        gates = jax.nn.sigmoid(gate_logits.astype(f32)).astype(x.dtype)
        g_a, g_b, g_c = jnp.split(gates, N_BRANCH, axis=-1)
        mixed = g_a * (o_a @ w_up_a[l]) + g_b * (o_b @ w_up_b[l]) + g_c * (o_c @ w_up_c[l])
        x = x + mixed @ w_out[l]
        h = rms_norm(x, ffn_norm[l])
        x = x + (jax.nn.silu(h @ w_ffn_gate[l]) * (h @ w_ffn_up[l])) @ w_ffn_down[l]

    return rms_norm(x, final_norm)
```

```python
import numpy as np
import concourse.bass as bass
import concourse.mybir as mybir
from concourse.bass_utils import run_bass_kernel_spmd
from contextlib import ExitStack

F32 = mybir.dt.float32
BF16 = mybir.dt.bfloat16
I32 = mybir.dt.int32
ALU = mybir.AluOpType
AF = mybir.ActivationFunctionType
AX = mybir.AxisListType

ENGS = ['pe', 'act', 'dve', 'pool', 'sp']
EPOCH = 30000
NDSEM = 24
NPSEM = 12
STRICT = True

T = 2048
D = 1024
NCH = 8
NG = 4
DEPTH = 4
DFF = 2816
NFF = 22
EPS = 1e-6
IN_COLS = 7176
OFF = dict(aq=0, ak=512, av=640, iq=768, ik=1280, iw=1344, bq=1352, bf=1864, bi=2376, bg=2888,
           cq=3400, ckv=3784, ckpe=4040, ga=4104, gb=5128, gc=6152)


VC_FINAL = 0
def VC_ATTN(l): return 8 + 22 * l
def VC_FFN(l): return 8 + 22 * l + 8
def VC_QN(l): return 8 + 22 * l + 16
def VC_KVN(l): return 8 + 22 * l + 19
def VC_HGN(l): return 8 + 22 * l + 21
VC_LB = 8 + 22 * DEPTH
NV = VC_LB + 16


def make_vecs(inp):
    v = np.zeros((128, NV), np.float32)
    v[:, 0:8] = inp["final_norm"].reshape(8, 128).T
    for l in range(DEPTH):
        v[:, VC_ATTN(l):VC_ATTN(l) + 8] = inp["attn_norm"][l].reshape(8, 128).T
        v[:, VC_FFN(l):VC_FFN(l) + 8] = inp["ffn_norm"][l].reshape(8, 128).T
        v[:, VC_QN(l):VC_QN(l) + 3] = inp["mla_q_norm"][l].reshape(3, 128).T
        v[:, VC_KVN(l):VC_KVN(l) + 2] = inp["mla_kv_norm"][l].reshape(2, 128).T
        v[:, VC_HGN(l)] = inp["hgrn_out_norm"][l]
        v[:, VC_LB + 4 * l:VC_LB + 4 * l + 4] = inp["hgrn_lb_logits"][l].reshape(4, 128).T
    return v


class Res:
    __slots__ = ('name', 'excl', 'w', 'r')

    def __init__(self, name, excl=False):
        self.name = name
        self.excl = excl
        self.w = None
        self.r = {}


class KB:
    def __init__(self, nc, es):
        self.nc = nc
        self.es = es
        self.e = {'pe': nc.tensor, 'act': nc.scalar, 'dve': nc.vector, 'pool': nc.gpsimd, 'sp': nc.sync}
        self.ops = []
        self.cnt = {e: 0 for e in ENGS}
        self.known = {e: {} for e in ENGS}
        self.sig = {e: set() for e in ENGS}
        self.dval = [0] * (NDSEM + NPSEM)
        self.dnext = 0
        self.pnext = 0
        self.out_tokens = []

    def _need(self, e, tok, waits):
        key, val = tok
        if self.known[e].get(key, 0) >= val:
            return
        self.known[e][key] = val
        waits.append(tok)
        if isinstance(key, str):
            self.sig[key].add(val)

    def op(self, eng, fn, reads=(), writes=(), dma=False, is_out=False):
        waits = []
        self.cnt[eng] += 1
        idx = self.cnt[eng]
        if dma:
            if eng == 'pool':
                s = NDSEM + self.pnext
                self.pnext = (self.pnext + 1) % NPSEM
            else:
                s = self.dnext
                self.dnext = (s + 1) % NDSEM
            prev = self.dval[s]
            if prev > 0:
                self._need(eng, (('d', s), prev), waits)
            self.dval[s] = prev + 16
            mytok = (('d', s), prev + 16)
        else:
            mytok = (eng, idx)
        for r in reads:
            if r.w is not None:
                k_ = r.w[0]
                if dma or k_ != eng or eng != 'pe':
                    self._need(eng, r.w, waits)
            if r.excl:
                for k_, v_ in r.r.items():
                    if dma or k_ != eng:
                        self._need(eng, (k_, v_), waits)
        strict = STRICT and eng != 'pe'
        for r in writes:
            if r.w is not None:
                k_ = r.w[0]
                if dma or k_ != eng or strict:
                    self._need(eng, r.w, waits)
            for k_, v_ in r.r.items():
                if dma or k_ != eng or strict:
                    self._need(eng, (k_, v_), waits)
        for r in reads:
            if r.excl:
                r.w = mytok
                r.r = {}
            else:
                r.r[mytok[0]] = mytok[1]
        for r in writes:
            r.w = mytok
            r.r = {}
        self.ops.append((eng, idx, fn, waits, mytok if dma else None))
        if is_out:
            self.out_tokens.append(mytok)
        return mytok

    def wait_tokens(self, eng, toks):
        waits = []
        for t in toks:
            self._need(eng, t, waits)
        self.cnt[eng] += 1
        self.ops.append((eng, self.cnt[eng], None, waits, None))

    def finish(self):
        nc = self.nc
        rank = {}
        esem = {}
        for e in ENGS:
            lst = sorted(self.sig[e])
            rank[e] = {v: i + 1 for i, v in enumerate(lst)}
            n = max(1, (len(lst) + EPOCH - 1) // EPOCH)
            esem[e] = [self.es.enter_context(nc.semaphore(f"s_{e}{i}")) for i in range(n)]
        dsem = [self.es.enter_context(nc.semaphore(f"d{i}")) for i in range(NDSEM + NPSEM)]
        nwait = 0
        for (eng, idx, fn, waits, dtok) in self.ops:
            E = self.e[eng]
            for key, val in waits:
                nwait += 1
                if isinstance(key, str):
                    rk = rank[key][val]
                    E.wait_ge(esem[key][(rk - 1) // EPOCH], (rk - 1) % EPOCH + 1)
                else:
                    E.wait_ge(dsem[key[1]], val)
            if fn is None:
                continue
            inst = fn(E)
            if dtok is not None:
                inst.then_inc(dsem[dtok[0][1]], 16)
            elif idx in rank[eng]:
                rk = rank[eng][idx]
                inst.then_inc(esem[eng][(rk - 1) // EPOCH], 1)
        self.stats = dict(cnt=dict(self.cnt), nwait=nwait, nsig={e: len(self.sig[e]) for e in ENGS})
        return self.stats


class Page:
    def __init__(self, ap32, name):
        self.f = ap32
        self.b = ap32.bitcast(BF16)
        self.q = [Res(f"{name}.{i}") for i in range(4)]

    @property
    def all(self):
        return self.q


A_SCALE = 128 ** -0.5
C_SCALE = 192 ** -0.5
B_SCALE = 128 ** -0.5
TWO_PI_HI = 6.28125
TWO_PI_LO = 2.0 * np.pi - 6.28125
NBIS = 11
NEGM = -30000.0


def build(n_layers=DEPTH, dbg=None, parts=('A', 'B', 'C', 'ffn'), layer0=0):
    dbg = dbg or {}
    nc = bass.Bass("TRN2", target_bir_lowering=False)
    es = ExitStack()
    k = KB(nc, es)

    def dr(name, shape, dt=F32, kind="ExternalInput"):
        return nc.dram_tensor(name, list(shape), dt, kind=kind).ap()

    x_d = dr("x", [T, D])
    out_d = dr("out", [T, D], kind="ExternalOutput")
    pos_d = dr("pos64", [64, T], I32)
    vecs_d = dr("vecs", [128, NV])
    relb_d = dr("relb", [128, 128])
    w_in_d = dr("w_in", [DEPTH, D, IN_COLS])
    w_up_d = {'A': dr("w_up_a", [DEPTH, 512, D]), 'B': dr("w_up_b", [DEPTH, 512, D]), 'C': dr("w_up_c", [DEPTH, 512, D])}
    w_out_d = dr("w_out", [DEPTH, D, D])
    w_qb_d = dr("mla_w_qb", [DEPTH, 384, 768])
    w_kvb_d = dr("mla_w_kvb", [DEPTH, 256, 1024])
    wg_d = dr("w_ffn_gate", [DEPTH, D, DFF])
    wu_d = dr("w_ffn_up", [DEPTH, D, DFF])
    wd_d = dr("w_ffn_down", [DEPTH, DFF, D])
    identf_d = dr("c_identf", [128, 128])
    cmaskf_d = dr("c_cmaskf", [128, 128])
    cmaskb_d = dr("c_cmaskb", [128, 128])
    caus01_d = dr("c_caus01", [128, 64])
    onehot_d = dr("c_onehot", [128, 64 * 128])
    pow2_d = dr("c_pow2", [128, NBIS])
    rope_d = dr("c_rope", [64, 2])
    dump_d = {n: dr("dbg_" + n, shp[0], shp[1], kind="ExternalOutput") for n, shp in dbg.items()}

    def sb(name, shape, dt=F32):
        return es.enter_context(nc.sbuf_tensor(name, list(shape), dt))

    XT = sb("XT", [128, NCH, T])
    XTr = [[Res(f"XT{c}.{g}") for g in range(NG)] for c in range(NCH)]
    HT = sb("HT", [128, NCH, T], BF16)
    HTr = [[Res(f"HT{c}.{g}") for g in range(NG)] for c in range(NCH)]
    NPG = 24
    GP = sb("GP", [128, NPG, 1024])
    pages = [Page(GP[:, i, :], f"pg{i}") for i in range(NPG)]
    PS = es.enter_context(nc.psum_tensor("PS", [128, 8, 512], F32))
    PSr = [Res(f"ps{i}", excl=True) for i in range(8)]
    ps_rr = {}

    def psum(tag, banks):
        i = ps_rr.get(tag, 0)
        ps_rr[tag] = i + 1
        b = banks[i % len(banks)]
        return PS[:, b, :], PSr[b]

    def small(name, shape, dt=F32):
        return sb("s_" + name, shape, dt), Res(name)

    identf, identf_r = small("identf", [128, 128])
    identb, identb_r = small("identb", [128, 128], BF16)
    onesb, onesb_r = small("onesb", [128, 128], BF16)
    gvec, gvec_r = small("gvec", [128, NV])
    cmaskf, cmaskf_r = small("cmaskf", [128, 128])
    cmaskb, cmaskb_r = small("cmaskb", [128, 128], BF16)
    caus01, caus01_r = small("caus01", [128, 64], BF16)
    pow2, pow2_r = small("pow2", [128, NBIS])
    ropec, ropec_r = small("ropec", [64, 2])
    biasT, biasT_r = small("biasT", [128, 8, 128], BF16)
    cosT, cosT_r = small("cosT", [64, T], BF16)
    sinT, sinT_r = small("sinT", [64, T], BF16)
    LB, LB_r = small("LB", [128, 16])
    OMLB, OMLB_r = small("OMLB", [128, 16])
    NOMLB, NOMLB_r = small("NOMLB", [128, 16])
    IW, IW_r = small("IW", [128, 16, 8])
    DL, DL_r = small("DL", [128, 32])
    S32s = [small(f"S32{i}", [128, 128]) for i in range(2)]
    SBF = [small(f"SBF{i}", [128, 128], BF16) for i in range(2)]
    bis, bis_r = small("bis", [128, 64])

    def DMA(q, out, in_, reads=(), writes=(), is_out=False):
        return k.op(q, lambda E: E.dma_start(out=out, in_=in_), reads=reads, writes=writes, dma=True, is_out=is_out)

    def MM(out, lhsT, rhs, start, stop, reads, writes, skip=False):
        k.op('pe', lambda E: E.matmul(out=out, lhsT=lhsT, rhs=rhs, start=start, stop=stop, skip_group_check=skip),
             reads=reads, writes=writes)

    def TR(out, in_, ident, reads, writes):
        k.op('pe', lambda E: E.transpose(out=out, in_=in_, identity=ident), reads=reads, writes=writes)

    def ACTV(out, in_, func, reads, writes, scale=1.0, bias=None, accum_out=None):
        kw = {}
        if bias is not None:
            kw['bias'] = bias
        if accum_out is not None:
            kw['accum_out'] = accum_out
        k.op('act', lambda E: E.activation(out=out, in_=in_, func=func, scale=scale, **kw), reads=reads, writes=writes)

    def TT(eng, out, in0, in1, op, reads, writes):
        k.op(eng, lambda E: E.tensor_tensor(out=out, in0=in0, in1=in1, op=op), reads=reads, writes=writes)

    def TS(eng, out, in0, s1, s2, op0, op1, reads, writes, accum_out=None):
        kw = {}
        if accum_out is not None:
            kw['accum_out'] = accum_out
        if op1 is None:
            k.op(eng, lambda E: E.tensor_scalar(out=out, in0=in0, scalar1=s1, scalar2=None, op0=op0, **kw),
                 reads=reads, writes=writes)
        else:
            k.op(eng, lambda E: E.tensor_scalar(out=out, in0=in0, scalar1=s1, scalar2=s2, op0=op0, op1=op1, **kw),
                 reads=reads, writes=writes)

    def STT(out, in0, scalar, in1, op0, op1, reads, writes):
        k.op('dve', lambda E: E.scalar_tensor_tensor(out=out, in0=in0, scalar=scalar, in1=in1, op0=op0, op1=op1),
             reads=reads, writes=writes)

    def CP(eng, out, in_, reads, writes):
        if eng == 'act':
            k.op('act', lambda E: E.activation(out=out, in_=in_, func=AF.Copy), reads=reads, writes=writes)
        else:
            k.op(eng, lambda E: E.tensor_copy(out=out, in_=in_), reads=reads, writes=writes)

    def RECIP(out, in_, reads, writes):
        k.op('dve', lambda E: E.reciprocal(out=out, in_=in_), reads=reads, writes=writes)

    def MEMSET(eng, ap, val, writes):
        k.op(eng, lambda E: E.memset(ap, val), writes=writes)

    def dump(name, ap, res):
        if name in dump_d:
            DMA('sp', dump_d[name], ap, reads=res, is_out=True)

    def slot(p0):
        base = GP[:, p0:p0 + 2, :].rearrange("p a b -> p (a b)").bitcast(BF16)
        return base, pages[p0].all + pages[p0 + 1].all

    def wload(p0, kc, ncols, parts_):
        assert kc * ncols <= 4096
        base, res = slot(p0)
        view = base[:, 0:kc * ncols].rearrange("p (k n) -> p k n", k=kc)
        for (c0, ncol, src) in parts_:
            DMA('pool', view[:, :, c0:c0 + ncol], src.rearrange("(k p) n -> p k n", p=128), writes=res)
        return view, res

    DMA('sp', identf[:], identf_d, writes=[identf_r])
    DMA('sp', gvec[:], vecs_d, writes=[gvec_r])
    DMA('sp', cmaskf[:], cmaskf_d, writes=[cmaskf_r])
    DMA('pool', cmaskb[:], cmaskb_d, writes=[cmaskb_r])
    DMA('pool', caus01[:], caus01_d, writes=[caus01_r])
    DMA('sp', pow2[:], pow2_d, writes=[pow2_r])
    DMA('sp', ropec[:], rope_d, writes=[ropec_r])
    CP('dve', identb[:], identf[:], [identf_r], [identb_r])
    MEMSET('dve', onesb[:], 1.0, [onesb_r])
    for i in range(16):
        pg = pages[20 + (i % 2)]
        DMA('sp' if i % 2 == 0 else 'act', pg.f, x_d[i * 128:(i + 1) * 128, :], writes=pg.all)
        g = i // 4
        for half in range(2):
            pap, pr = psum('tr', [0, 1])
            for j in range(4):
                c = half * 4 + j
                TR(pap[:, j * 128:(j + 1) * 128], pg.f[:, c * 128:(c + 1) * 128], identf[:], pg.all + [identf_r], [pr])
            CP('act', XT[:, half * 4:half * 4 + 4, i * 128:(i + 1) * 128],
               pap.rearrange("p (c t) -> p c t", c=4), [pr], [XTr[c][g] for c in range(half * 4, half * 4 + 4)])

    def pairf(p0):
        return GP[:, p0:p0 + 2, :].rearrange("p a b -> p (a b)"), pages[p0].all + pages[p0 + 1].all

    PIv, PI_r = pairf(0)
    Av, A_r = pairf(2)
    Nv, N_r = pairf(4)
    Rv, R_r = pairf(6)
    PIi = PIv.bitcast(I32)
    DMA('sp', PIi[0:64, :], pos_d, writes=PI_r)
    CP('dve', Av[0:64, :], PIi[0:64, :], PI_r, A_r)
    TS('dve', Av[0:64, :], Av[0:64, :], ropec[:, 0:1], None, ALU.mult, None, A_r + [ropec_r], A_r)
    for which in (0, 1):
        shift = 0.0 if which == 0 else 0.5 * np.pi
        TS('dve', Nv[0:64, :], Av[0:64, :], 1.0 / (2 * np.pi), shift / (2 * np.pi), ALU.mult, ALU.add, A_r, N_r)
        CP('dve', PIi[0:64, :], Nv[0:64, :], N_r, PI_r)
        CP('dve', Nv[0:64, :], PIi[0:64, :], PI_r, N_r)
        STT(Rv[0:64, :], Nv[0:64, :], -TWO_PI_HI, Av[0:64, :], ALU.mult, ALU.add, N_r + A_r, R_r)
        STT(Rv[0:64, :], Nv[0:64, :], -TWO_PI_LO, Rv[0:64, :], ALU.mult, ALU.add, N_r + R_r, R_r)
        if which == 1:
            TS('dve', Rv[0:64, :], Rv[0:64, :], shift, None, ALU.add, None, R_r, R_r)
        TS('dve', Rv[0:64, :], Rv[0:64, :], -3.1415925, 3.1415925, ALU.max, ALU.min, R_r, R_r)
        if which == 0:
            ACTV(Nv[0:64, :], Rv[0:64, :], AF.Sin, R_r, N_r)
            TS('dve', sinT[:], Nv[0:64, :], ropec[:, 1:2], None, ALU.mult, None, N_r + [ropec_r], [sinT_r])
        else:
            ACTV(cosT[:], Rv[0:64, :], AF.Sin, R_r, [cosT_r])
    dump('cosT', cosT[:], [cosT_r])
    dump('sinT', sinT[:], [sinT_r])

    OHv = GP[:, 8:12, :].rearrange("p a b -> p (a b)").bitcast(BF16).rearrange("p (n s) -> p n s", s=128)
    OH_r = pages[8].all + pages[9].all + pages[10].all + pages[11].all
    DMA('pool', OHv, onehot_d.rearrange("p (n s) -> p n s", s=128), writes=OH_r)
    relb = pages[13].f[:, 0:128]
    relb_r = pages[13].q[0]
    DMA('sp', relb, relb_d, writes=[relb_r])
    ACC = pages[12].f.rearrange("p (n s) -> p n s", s=128)
    ACC_r = pages[12].all
    accr = [Res(f"acc{j}") for j in range(8)]
    MEMSET('dve', pages[12].f, 0.0, ACC_r + accr)
    for b in range(32):
        for h in range(4):
            for tbl in range(2):
                hj = h * 2 + tbl
                STT(ACC[:, hj, :], OHv[:, tbl * 32 + b, :], relb[:, b * 4 + h:b * 4 + h + 1], ACC[:, hj, :],
                    ALU.mult, ALU.add, OH_r + [accr[hj], relb_r], [accr[hj]])
    for h in range(4):
        for tbl in range(2):
            hj = h * 2 + tbl
            TS('dve', biasT[:, hj, :], ACC[:, hj, :], relb[:, 31 * 4 + h:31 * 4 + h + 1], None, ALU.subtract, None,
               [accr[hj], relb_r], [biasT_r])
    MEMSET('dve', bis[:, 0:1], 0.0, ACC_r + accr + [bis_r])
    dump('biasT', biasT[:], [biasT_r])

    lbs, lbs_r = small("lbs", [128, 64])
    Lg = lambda l: gvec[:, VC_LB + 4 * l:VC_LB + 4 * l + 4]
    TT('dve', lbs[:, 0:4], Lg(0), Lg(1), ALU.max, [gvec_r], [lbs_r])
    TT('dve', lbs[:, 0:4], lbs[:, 0:4], Lg(2), ALU.max, [gvec_r, lbs_r], [lbs_r])
    TT('dve', lbs[:, 0:4], lbs[:, 0:4], Lg(3), ALU.max, [gvec_r, lbs_r], [lbs_r])
    for l in range(4):
        TT('dve', lbs[:, 8 + 4 * l:12 + 4 * l], Lg(l), lbs[:, 0:4], ALU.subtract, [gvec_r, lbs_r], [lbs_r])
    ACTV(lbs[:, 8:24], lbs[:, 8:24], AF.Exp, [lbs_r], [lbs_r])
    TT('dve', lbs[:, 4:8], lbs[:, 8:12], lbs[:, 12:16], ALU.add, [lbs_r], [lbs_r])
    TT('dve', lbs[:, 4:8], lbs[:, 4:8], lbs[:, 16:20], ALU.add, [lbs_r], [lbs_r])
    TT('dve', lbs[:, 4:8], lbs[:, 4:8], lbs[:, 20:24], ALU.add, [lbs_r], [lbs_r])
    RECIP(lbs[:, 4:8], lbs[:, 4:8], [lbs_r], [lbs_r])
    for l in range(4):
        TT('dve', lbs[:, 8 + 4 * l:12 + 4 * l], lbs[:, 8 + 4 * l:12 + 4 * l], lbs[:, 4:8], ALU.mult, [lbs_r], [lbs_r])
    MEMSET('dve', LB[:, 0:4], 0.0, [LB_r])
    CP('dve', LB[:, 4:8], lbs[:, 12:16], [lbs_r], [LB_r])
    TT('dve', LB[:, 8:12], LB[:, 4:8], lbs[:, 16:20], ALU.add, [lbs_r, LB_r], [LB_r])
    TT('dve', LB[:, 12:16], LB[:, 8:12], lbs[:, 20:24], ALU.add, [lbs_r, LB_r], [LB_r])
    TS('dve', OMLB[:], LB[:], -1.0, 1.0, ALU.mult, ALU.add, [LB_r], [OMLB_r])
    TS('dve', NOMLB[:], LB[:], 1.0, None, ALU.subtract, None, [LB_r], [NOMLB_r])
    dump('LB', LB[:], [LB_r])

    def rbc_from(sq_src, src_res, nchunks, inv_n, g, sq_pgs, rbc_ap, rbc_res):
        pap, pr = psum('nrm', [6, 7])
        for c in range(nchunks):
            sp = sq_pgs[c % len(sq_pgs)]
            qi = (c // len(sq_pgs)) % 4
            ACTV(sp.b[:, qi * 512:(qi + 1) * 512], sq_src(c), AF.Square, src_res(c), [sp.q[qi]])
            MM(pap, onesb[:], sp.b[:, qi * 512:(qi + 1) * 512], c == 0, c == nchunks - 1, [sp.q[qi], onesb_r], [pr])
        ACTV(rbc_ap, pap, AF.Ln, [pr], rbc_res, scale=inv_n, bias=EPS)
        ACTV(rbc_ap, rbc_ap, AF.Exp, rbc_res, rbc_res, scale=-0.5)

    def norm_to_HT(gcol, sqp, rbp):
        for g in range(NG):
            rb = pages[rbp]
            hq = (g % 2) * 2
            rap = rb.f[:, (g % 2) * 512:(g % 2) * 512 + 512]
            rres = [rb.q[hq], rb.q[hq + 1]]
            rbc_from(lambda c: XT[:, c, g * 512:(g + 1) * 512], lambda c: [XTr[c][g]], NCH, 1.0 / D, g,
                     [pages[sqp], pages[sqp + 1]], rap, rres)
            for c in range(NCH):
                STT(HT[:, c, g * 512:(g + 1) * 512], XT[:, c, g * 512:(g + 1) * 512], gvec[:, gcol + c:gcol + c + 1], rap,
                    ALU.mult, ALU.mult, [XTr[c][g], gvec_r] + rres, [HTr[c][g]])
    at_rr = [0]

    def attn_group(i0, nqb, wide_terms, blk_terms, v_of_j, out_ap, out_res, pt_bufs, rs_bufs, rs_in_ot=False):
        NQ = nqb * 128
        jlast = i0 + nqb - 1
        per_bank = 4 // nqb
        OTp, OTr = psum('ot', [4, 5])
        if rs_in_ot:
            assert NQ <= 256
            RSp, RSr = OTp[:, 256:512], OTr
        else:
            RSp, RSr = psum('rs', [6, 7])
        chunks = []
        j = 0
        while j <= jlast:
            nj = min(per_bank, jlast - j + 1)
            chunks.append((j, nj))
            j += nj

        def stage_qk(j, nj):
            STp, STr = psum('st', [2, 3])
            pt, ptr = pt_bufs[at_rr[0] % len(pt_bufs)]
            at_rr[0] += 1
            spans = []
            for jj in range(nj):
                jk = j + jj
                ia = max(i0, jk)
                off = (ia - i0) * 128
                wd = NQ - off
                base = jj * NQ
                tl = [(l_ap, l_res, r_ap[:, off:NQ], r_res, base + off, wd) for (l_ap, l_res, r_ap, r_res) in wide_terms(jk)]
                for i in range(ia, i0 + nqb):
                    for (l_ap, l_res, r_ap, r_res) in blk_terms(jk, i):
                        tl.append((l_ap, l_res, r_ap, r_res, base + (i - i0) * 128, 128))
                for ti, (l_ap, l_res, r_ap, r_res, c0, w) in enumerate(tl):
                    MM(STp[:, c0:c0 + w], l_ap, r_ap, ti == 0, ti == len(tl) - 1, l_res + r_res, [STr])
                spans.append((jk, base + off, wd, off))
            lo = spans[0][1]
            hi = spans[-1][1] + spans[-1][2]
            if len(spans) == 1 or all(sp[3] == 0 for sp in spans):
                ACTV(pt[:, lo:hi], STp[:, lo:hi], AF.Exp, [STr], [ptr])
            else:
                for (jk, c0, w, off) in spans:
                    ACTV(pt[:, c0:c0 + w], STp[:, c0:c0 + w], AF.Exp, [STr], [ptr])
            return pt, ptr, spans

        def stage_pv(pt, ptr, spans):
            for (jk, c0, w, off) in spans:
                v_ap, v_res = v_of_j(jk)
                if rs_in_ot:
                    MM(OTp[:, off:NQ], v_ap, pt[:, c0:c0 + w], jk == 0, False, v_res + [ptr], [OTr], skip=True)
                    MM(RSp[:, off:NQ], onesb[:], pt[:, c0:c0 + w], False, jk == jlast, [onesb_r, ptr], [RSr], skip=True)
                else:
                    MM(OTp[:, off:NQ], v_ap, pt[:, c0:c0 + w], jk == 0, jk == jlast, v_res + [ptr], [OTr])
                    MM(RSp[:, off:NQ], onesb[:], pt[:, c0:c0 + w], jk == 0, jk == jlast, [onesb_r, ptr], [RSr])

        pend = stage_qk(*chunks[0])
        for ci in range(1, len(chunks)):
            nxt = stage_qk(*chunks[ci])
            stage_pv(*pend)
            pend = nxt
        stage_pv(*pend)
        rs_ap, rs_res = rs_bufs[at_rr[0] % len(rs_bufs)]
        ACTV(rs_ap[:, 0:NQ], RSp[:, 0:NQ], AF.Ln, [RSr], rs_res)
        ACTV(rs_ap[:, 0:NQ], rs_ap[:, 0:NQ], AF.Exp, rs_res, rs_res, scale=-1.0)
        TT('dve', out_ap, OTp[:, 0:NQ], rs_ap[:, 0:NQ], ALU.mult, ([OTr] if rs_in_ot else [OTr]) + rs_res, out_res)

    def guo(l, br, OX, slots5, mpairs, tmp_pg):
        goff = {'A': OFF['ga'], 'B': OFF['gb'], 'C': OFF['gc']}[br]
        wup, wup_r = wload(slots5[0], 4, 1024, [(0, 1024, w_up_d[br][l, :, :])])
        wga, wga_r = wload(slots5[1], 8, 512, [(0, 512, w_in_d[l, :, goff:goff + 512])])
        wgb, wgb_r = wload(slots5[2], 8, 512, [(0, 512, w_in_d[l, :, goff + 512:goff + 1024])])
        woa, woa_r = wload(slots5[3], 8, 512, [(0, 512, w_out_d[l, :, 0:512])])
        wob, wob_r = wload(slots5[4], 8, 512, [(0, 512, w_out_d[l, :, 512:1024])])
        for g in range(NG):
            mp = mpairs[g % len(mpairs)]
            for n in range(NCH):
                wg_, wg_r_ = (wga, wga_r) if n < 4 else (wgb, wgb_r)
                nn = n % 4
                pg_, pgr = psum('mm', [0, 1])
                for c in range(NCH):
                    MM(pg_, wg_[:, c, nn * 128:(nn + 1) * 128], HT[:, c, g * 512:(g + 1) * 512], c == 0, c == NCH - 1,
                       wg_r_ + [HTr[c][g]], [pgr])
                pu_, pur = psum('st', [2, 3])
                for c in range(4):
                    MM(pu_, wup[:, c, n * 128:(n + 1) * 128], OX[c].b[:, g * 512:(g + 1) * 512], c == 0, c == 3,
                       wup_r + [OX[c].q[g]], [pur])
                tq = n % 4
                ACTV(tmp_pg.b[:, tq * 512:(tq + 1) * 512], pg_, AF.Sigmoid, [pgr], [tmp_pg.q[tq]])
                mpg = mp[n // 4]
                TT('dve', mpg.b[:, (n % 4) * 512:(n % 4) * 512 + 512], tmp_pg.b[:, tq * 512:(tq + 1) * 512], pu_, ALU.mult,
                   [tmp_pg.q[tq], pur], [mpg.q[n % 4]])
            for n2 in range(NCH):
                wo_, wo_r_ = (woa, woa_r) if n2 < 4 else (wob, wob_r)
                nn = n2 % 4
                py, pyr = psum('ot', [4, 5])
                for c in range(NCH):
                    mpg = mp[c // 4]
                    MM(py, wo_[:, c, nn * 128:(nn + 1) * 128], mpg.b[:, (c % 4) * 512:(c % 4) * 512 + 512], c == 0,
                       c == NCH - 1, wo_r_ + [mpg.q[c % 4]], [pyr])
                TT('dve', XT[:, n2, g * 512:(g + 1) * 512], XT[:, n2, g * 512:(g + 1) * 512], py, ALU.add,
                   [XTr[n2][g], pyr], [XTr[n2][g]])

    def branch_C(l):
        KVN = [pages[0], pages[1]]
        KNT = [pages[2 + h] for h in range(4)]
        VCv = GP[:, 6:10, :].rearrange("p a b -> p (a b)").bitcast(BF16).rearrange("p (i h d) -> p i h d", i=16, h=4)
        VC_r = [pages[6 + a].all for a in range(4)]
        KPET = pages[10]
        QN = [pages[11], pages[12], pages[13]]
        OC = [pages[14 + h] for h in range(4)]
        SM = pages[18]
        RAW = pages[19]
        W1, W1_r = wload(20, 8, 384, [(0, 256, w_in_d[l, :, OFF['ckv']:OFF['ckv'] + 256]),
                                      (256, 64, w_in_d[l, :, OFF['ckpe']:OFF['ckpe'] + 64]),
                                      (320, 32, w_in_d[l, :, OFF['ckpe'] + 32:OFF['ckpe'] + 64]),
                                      (352, 32, w_in_d[l, :, OFF['ckpe']:OFF['ckpe'] + 32])])
        W2, W2_r = wload(22, 2, 1024, [(0, 1024, w_kvb_d[l, :, :])])
        W2v = W2.rearrange("p k (h two d) -> p k h two d", h=4, two=2)
        for g in range(NG):
            gs = slice(g * 512, (g + 1) * 512)
            raws = []
            for c2 in range(2):
                pp, ppr = psum('mmw', [0, 1, 2, 3, 4, 5])
                for c in range(NCH):
                    MM(pp, W1[:, c, c2 * 128:(c2 + 1) * 128], HT[:, c, gs], c == 0, c == NCH - 1, W1_r + [HTr[c][g]], [ppr])
                rap = RAW.f[:, c2 * 512:(c2 + 1) * 512]
                rres = [RAW.q[2 * c2], RAW.q[2 * c2 + 1]]
                CP('act', rap, pp, [ppr], rres)
                raws.append((rap, rres))
            rbp = pages[11]
            rb = rbp.f[:, 0:512]
            rb_res = [rbp.q[0], rbp.q[1]]
            rbc_from(lambda c: raws[c][0], lambda c: raws[c][1], 2, 1.0 / 256, g, [pages[12]], rb, rb_res)
            for c2 in range(2):
                STT(KVN[c2].b[:, gs], raws[c2][0], gvec[:, VC_KVN(l) + c2:VC_KVN(l) + c2 + 1], rb, ALU.mult, ALU.mult,
                    raws[c2][1] + [gvec_r] + rb_res, [KVN[c2].q[g]])
            pa, par = psum('mmw', [0, 1, 2, 3, 4, 5])
            for c in range(NCH):
                MM(pa[0:64, :], W1[:, c, 256:320], HT[:, c, gs], c == 0, c == NCH - 1, W1_r + [HTr[c][g]], [par])
            pb, pbr = psum('mmw', [0, 1, 2, 3, 4, 5])
            for c in range(NCH):
                MM(pb[0:64, :], W1[:, c, 320:384], HT[:, c, gs], c == 0, c == NCH - 1, W1_r + [HTr[c][g]], [pbr])
            t1 = pages[13].f[0:64, 0:512]
            t1r = [pages[13].q[0], pages[13].q[1]]
            t2 = pages[13].f[0:64, 512:1024]
            t2r = [pages[13].q[2], pages[13].q[3]]
            TT('dve', t1, pa[0:64, :], cosT[:, gs], ALU.mult, [par, cosT_r], t1r)
            TT('dve', t2, pb[0:64, :], sinT[:, gs], ALU.mult, [pbr, sinT_r], t2r)
            TT('dve', KPET.b[0:64, gs], t1, t2, ALU.add, t1r + t2r, [KPET.q[g]])
            for h in range(4):
                pp, ppr = psum('mmw', [0, 1, 2, 3, 4, 5])
                for c2 in range(2):
                    MM(pp, W2v[:, c2, h, 0, :], KVN[c2].b[:, gs], c2 == 0, c2 == 1, W2_r + [KVN[c2].q[g]], [ppr])
                CP('act', KNT[h].b[:, gs], pp, [ppr], [KNT[h].q[g]])
            for ti in range(4):
                i = g * 4 + ti
                pp, ppr = psum('mmw', [0, 1, 2, 3, 4, 5])
                for c2 in range(2):
                    MM(pp.rearrange("p (h d) -> p h d", h=4), KVN[c2].b[:, i * 128:(i + 1) * 128], W2v[:, c2, :, 1, :],
                       c2 == 0, c2 == 1, W2_r + [KVN[c2].q[g]], [ppr])
                CP('dve', VCv[:, i, :, :], pp.rearrange("p (h d) -> p h d", h=4), [ppr], VC_r[i // 4])
        dump('kpeT', KPET.b[0:64, :], KPET.all)
        dump('knT0', KNT[0].b, KNT[0].all)
        W3, W3_r = wload(20, 8, 384, [(0, 384, w_in_d[l, :, OFF['cq']:OFF['cq'] + 384])])
        RAW2 = pages[18]
        for g in range(NG):
            gs = slice(g * 512, (g + 1) * 512)
            raws = []
            for c3 in range(3):
                pp, ppr = psum('mmw', [0, 1, 2, 3, 4, 5])
                for c in range(NCH):
                    MM(pp, W3[:, c, c3 * 128:(c3 + 1) * 128], HT[:, c, gs], c == 0, c == NCH - 1, W3_r + [HTr[c][g]], [ppr])
                pgx = RAW if c3 < 2 else RAW2
                cc = c3 % 2
                rap = pgx.f[:, cc * 512:(cc + 1) * 512]
                rres = [pgx.q[2 * cc], pgx.q[2 * cc + 1]]
                CP('act', rap, pp, [ppr], rres)
                raws.append((rap, rres))
            rbp = pages[0]
            rb = rbp.f[:, 0:512]
            rb_res = [rbp.q[0], rbp.q[1]]
            rbc_from(lambda c: raws[c][0], lambda c: raws[c][1], 3, 1.0 / 384, g, [pages[1]], rb, rb_res)
            for c3 in range(3):
                STT(QN[c3].b[:, gs], raws[c3][0], gvec[:, VC_QN(l) + c3:VC_QN(l) + c3 + 1], rb, ALU.mult, ALU.mult,
                    raws[c3][1] + [gvec_r] + rb_res, [QN[c3].q[g]])
        qb = w_qb_d[l]
        W4, W4_r = wload(20, 3, 512, [(h * 128, 128, qb[:, h * 192:h * 192 + 128]) for h in range(4)])
        p5 = []
        for h in range(4):
            p5.append((h * 64, 64, qb[:, h * 192 + 128:h * 192 + 192]))
            p5.append((256 + h * 64, 32, qb[:, h * 192 + 160:h * 192 + 192]))
            p5.append((256 + h * 64 + 32, 32, qb[:, h * 192 + 128:h * 192 + 160]))
        W5, W5_r = wload(22, 3, 512, p5)
        QNG = pages[18]
        QPG = pages[0]
        QnTg = QNG.b.rearrange("p (h t) -> p h t", h=4)
        QpeTg = QPG.b[0:64, :].rearrange("p (h t) -> p h t", h=4)
        PTP = pages[1]
        pt_bufs = [(PTP.b[:, 0:512], PTP.q[0]), (PTP.b[:, 512:1024], PTP.q[1])]
        rs_bufs = [(PTP.f[:, 512:1024], [PTP.q[2], PTP.q[3]])]
        for G in range(4):
            gs = slice(G * 512, (G + 1) * 512)
            for h in range(4):
                pp, ppr = psum('mm', [0, 1])
                for c3 in range(3):
                    MM(pp, W4[:, c3, h * 128:(h + 1) * 128], QN[c3].b[:, gs], c3 == 0, c3 == 2, W4_r + [QN[c3].q[G]], [ppr])
                ACTV(QnTg[:, h, :], pp, AF.Copy, [ppr], [QNG.q[h]], scale=C_SCALE)
                pa, par = psum('mm', [0, 1])
                for c3 in range(3):
                    MM(pa[0:64, :], W5[:, c3, h * 64:(h + 1) * 64], QN[c3].b[:, gs], c3 == 0, c3 == 2, W5_r + [QN[c3].q[G]], [par])
                pb, pbr = psum('mm', [0, 1])
                for c3 in range(3):
                    MM(pb[0:64, :], W5[:, c3, 256 + h * 64:256 + (h + 1) * 64], QN[c3].b[:, gs], c3 == 0, c3 == 2,
                       W5_r + [QN[c3].q[G]], [pbr])
                t1 = RAW.f[0:64, 0:512]
                t1r = [RAW.q[0], RAW.q[1]]
                t2 = RAW.f[0:64, 512:1024]
                t2r = [RAW.q[2], RAW.q[3]]
                STT(t1, pa[0:64, :], C_SCALE, cosT[:, gs], ALU.mult, ALU.mult, [par, cosT_r], t1r)
                STT(t2, pb[0:64, :], C_SCALE, sinT[:, gs], ALU.mult, ALU.mult, [pbr, sinT_r], t2r)
                TT('dve', QpeTg[:, h, :], t1, t2, ALU.add, t1r + t2r, [QPG.q[h]])
            for h in range(4):
                def wide(j, h=h):
                    ks = slice(j * 128, (j + 1) * 128)
                    return [(KNT[h].b[:, ks], [KNT[h].q[j // 4]], QnTg[:, h, :], [QNG.q[h]]),
                            (KPET.b[0:64, ks], [KPET.q[j // 4]], QpeTg[:, h, :], [QPG.q[h]])]

                def blk(j, i):
                    if j == i:
                        return [(cmaskb[:], [cmaskb_r], identb[:], [identb_r])]
                    return []
                attn_group(G * 4, 4, wide, blk, lambda j, h=h: (VCv[:, j, h, :], VC_r[j // 4]),
                           OC[h].b[:, gs], [OC[h].q[G]], pt_bufs, rs_bufs)
        for h in range(4):
            dump(f'ocT{h}', OC[h].b, OC[h].all)
        guo(l, 'C', OC, [20, 22, 0, 2, 4], [(pages[6], pages[7]), (pages[8], pages[9])], pages[10])
    def branch_A(l):
        KT = pages[0]
        VTv = pages[1].b.rearrange("p (i d) -> p i d", i=16)
        IKT = pages[2]
        SCv = GP[:, 3:5, :].rearrange("p a b -> p (a b)")
        SC_r = pages[3].all + pages[4].all
        OA = [pages[6 + h] for h in range(4)]
        SM = pages[10]
        RH = pages[11]
        W_aq, W_aq_r = wload(12, 8, 512, [(0, 512, w_in_d[l, :, OFF['aq']:OFF['aq'] + 512])])
        W_iq, W_iq_r = wload(14, 8, 512, [(0, 512, w_in_d[l, :, OFF['iq']:OFF['iq'] + 512])])
        W_kv, W_kv_r = wload(16, 8, 256, [(0, 256, w_in_d[l, :, OFF['ak']:OFF['ak'] + 256])])
        W_ix, W_ix_r = wload(18, 8, 136, [(0, 64, w_in_d[l, :, OFF['ik']:OFF['ik'] + 64]),
                                          (64, 64, w_in_d[l, :, OFF['ik']:OFF['ik'] + 64]),
                                          (128, 8, w_in_d[l, :, OFF['iw']:OFF['iw'] + 8])])
        for g in range(NG):
            gs = slice(g * 512, (g + 1) * 512)
            pp, ppr = psum('mmw', [0, 1, 2, 3, 4, 5])
            for c in range(NCH):
                MM(pp, W_kv[:, c, 0:128], HT[:, c, gs], c == 0, c == NCH - 1, W_kv_r + [HTr[c][g]], [ppr])
            CP('act', KT.b[:, gs], pp, [ppr], [KT.q[g]])
            pp, ppr = psum('mmw', [0, 1, 2, 3, 4, 5])
            for c in range(NCH):
                MM(pp, W_ix[:, c, 0:128], HT[:, c, gs], c == 0, c == NCH - 1, W_ix_r + [HTr[c][g]], [ppr])
            CP('dve', IKT.b[:, gs], pp, [ppr], [IKT.q[g]])
            pp, ppr = psum('mmw', [0, 1, 2, 3, 4, 5])
            for ti in range(4):
                i = g * 4 + ti
                for c in range(NCH):
                    MM(pp[:, ti * 128:(ti + 1) * 128], HT[:, c, i * 128:(i + 1) * 128], W_kv[:, c, 128:256], c == 0,
                       c == NCH - 1, W_kv_r + [HTr[c][g]], [ppr])
            CP('act', VTv[:, g * 4:(g + 1) * 4, :], pp.rearrange("p (i d) -> p i d", i=4), [ppr], [pages[1].q[g]])
            pp, ppr = psum('mmw', [0, 1, 2, 3, 4, 5])
            for ti in range(4):
                i = g * 4 + ti
                for c in range(NCH):
                    MM(pp[:, ti * 8:(ti + 1) * 8], HT[:, c, i * 128:(i + 1) * 128], W_ix[:, c, 128:136], c == 0,
                       c == NCH - 1, W_ix_r + [HTr[c][g]], [ppr])
            CP('dve', IW[:, g * 4:(g + 1) * 4, :], pp[:, 0:32].rearrange("p (i d) -> p i d", i=4), [ppr], [IW_r])
        QTi = SM.b[:, 0:512].rearrange("p (h t) -> p h t", h=4)
        IQTi = SM.b[:, 512:1024].rearrange("p (h t) -> p h t", h=4)
        pt_bufs = [(SM.b[:, 1024:1536], SM.q[2]), (SM.b[:, 1536:2048], SM.q[3])]
        SCs = [(SCv, SC_r),
               (GP[:, 16:18, :].rearrange("p a b -> p (a b)"), pages[16].all + pages[17].all)]
        MBs = [pages[5], pages[21], pages[18], pages[19]]
        IQs = [(SM.b[:, 512:1024].rearrange("p (h t) -> p h t", h=4), SM.q[1]),
               (pages[22].b[:, 0:512].rearrange("p (h t) -> p h t", h=4), pages[22].q[0])]
        RSP = pages[20]
        rs_bufs = [(RSP.f[:, 0:256], [RSP.q[0]]), (RSP.f[:, 256:512], [RSP.q[1]])]
        BW = 32
        bR = [dict(bnd=Res(f"bis{sl}.bnd"), mid=Res(f"bis{sl}.mid"), cnt=Res(f"bis{sl}.cnt"), tmp=Res(f"bis{sl}.tmp"),
                   stp=Res(f"bis{sl}.stp"), thr=Res(f"bis{sl}.thr")) for sl in range(2)]

        def stage2(i, MB):
            g = i // 4
            ts_ = slice(i * 128, (i + 1) * 128)
            pp, ppr = psum('mm', [0, 1])
            for h in range(4):
                for c in range(NCH):
                    MM(pp[:, h * 128:(h + 1) * 128], W_aq[:, c, h * 128:(h + 1) * 128], HT[:, c, ts_], c == 0, c == NCH - 1,
                       W_aq_r + [HTr[c][g]], [ppr])
            ACTV(QTi, pp.rearrange("p (h t) -> p h t", h=4), AF.Copy, [ppr], [SM.q[0]], scale=A_SCALE)
            yield
            for h in range(4):
                def wide(j, h=h):
                    ks = slice(j * 128, (j + 1) * 128)
                    return [(KT.b[:, ks], [KT.q[j // 4]], QTi[:, h, :], [SM.q[0]])]

                def blk(j, i_, h=h, MB=MB):
                    ks = slice(j * 128, (j + 1) * 128)
                    tl = [(MB.b[:, ks], [MB.q[j // 4]], identb[:], [identb_r])]
                    if j == i_:
                        tl.append((biasT[:, h * 2, :], [biasT_r], identb[:], [identb_r]))
                    elif j == i_ - 1:
                        tl.append((biasT[:, h * 2 + 1, :], [biasT_r], identb[:], [identb_r]))
                    return tl
                attn_group(i, 1, wide, blk, lambda j: (VTv[:, j, :], [pages[1].q[j // 4]]), OA[h].b[:, ts_], [OA[h].q[g]],
                           pt_bufs, rs_bufs, rs_in_ot=True)
                yield

        RH2 = pages[23]
        RBs = [(RH.f[:, 0:512], [RH.q[0], RH.q[1]]), (RH.f[:, 512:1024], [RH.q[2], RH.q[3]]),
               (RH2.f[:, 0:512], [RH2.q[0], RH2.q[1]]), (RH2.f[:, 512:1024], [RH2.q[2], RH2.q[3]])]
        rb_rr = [0]

        def scores(i, slot, other):
            g = i // 4
            ts_ = slice(i * 128, (i + 1) * 128)
            W = (i + 1) * 128
            nq = (W + 255) // 256
            SCv_, SC_r_ = SCs[slot]
            IQTi_, iq_r = IQs[slot]
            b0 = slot * BW
            pp, ppr = psum('mm', [0, 1])
            for j in range(4):
                for c in range(NCH):
                    MM(pp[:, j * 128:(j + 1) * 128], W_iq[:, c, j * 128:(j + 1) * 128], HT[:, c, ts_], c == 0, c == NCH - 1,
                       W_iq_r + [HTr[c][g]], [ppr])
            CP('dve', IQTi_, pp.rearrange("p (h t) -> p h t", h=4), [ppr], [iq_r])
            for hh in range(8):
                po = (hh % 2) * 64
                for k0 in range(0, W, 512):
                    kw = min(512, W - k0)
                    lp, lpr = psum('lg', [6, 7, 0, 1])
                    MM(lp[:, 0:kw], IQTi_[po:po + 64, hh // 2, :], IKT.b[po:po + 64, k0:k0 + kw], True, True,
                       [iq_r] + [IKT.q[kq] for kq in range(k0 // 512, (k0 + kw + 511) // 512)], [lpr])
                    rhf, rhr = RBs[rb_rr[0] % 4]
                    rb_rr[0] += 1
                    rh = rhf[:, 0:kw]
                    ACTV(rh, lp[:, 0:kw], AF.Relu, [lpr], rhr)
                    scr = SC_r_[k0 // 256:(k0 + kw + 255) // 256]
                    if hh == 0:
                        TS('dve', SCv_[:, k0:k0 + kw], rh, IW[:, i, 0:1], None, ALU.mult, None, rhr + [IW_r], scr)
                    else:
                        STT(SCv_[:, k0:k0 + kw], rh, IW[:, i, hh:hh + 1], SCv_[:, k0:k0 + kw], ALU.mult, ALU.add,
                            rhr + [IW_r] + scr, scr)
                if other is not None and hh % 4 == 3:
                    next(other, None)
            scw = SC_r_[0:nq]
            if i >= 2:
                k.op('dve', lambda E: E.tensor_reduce(out=bis[:, b0:b0 + 1], in_=SCv_[:, 0:W], axis=AX.X, op=ALU.max),
                     reads=scw, writes=[bR[slot]['bnd']])
                k.op('dve', lambda E: E.tensor_reduce(out=bis[:, b0 + 1:b0 + 2], in_=SCv_[:, 0:W], axis=AX.X, op=ALU.min),
                     reads=scw, writes=[bR[slot]['bnd']])
            TT('dve', SCv_[:, i * 128:W], SCv_[:, i * 128:W], cmaskf[:], ALU.add, [cmaskf_r] + SC_r_[(i * 128) // 256:nq],
               SC_r_[(i * 128) // 256:nq])

        def bis_init(i, slot):
            b0 = slot * BW
            R = bR[slot]
            sg = 1.0 if slot == 0 else -1.0
            TT('dve', bis[:, b0 + 2:b0 + 3], bis[:, b0:b0 + 1], bis[:, b0 + 1:b0 + 2], ALU.subtract, [R['bnd']], [R['tmp']])
            TS('dve', bis[:, b0 + 8:b0 + 8 + NBIS], pow2[:], bis[:, b0 + 2:b0 + 3], sg, ALU.mult, ALU.mult,
               [pow2_r, R['tmp']], [R['stp']])
            STT(bis[:, b0 + 3:b0 + 4], bis[:, b0 + 1:b0 + 2], -sg, bis[:, b0 + 8:b0 + 9], ALU.mult, ALU.subtract,
                [R['bnd'], R['stp']], [R['mid']])

        def bis_pass(i, slot, MB):
            W = (i + 1) * 128
            nq = (W + 255) // 256
            SCv_, SC_r_ = SCs[slot]
            b0 = slot * BW
            R = bR[slot]
            if slot == 0:
                ACTV(MB.b[:, 0:W], SCv_[:, 0:W], AF.Sign, SC_r_[0:nq] + [R['mid']], MB.all + [R['cnt']],
                     bias=bis[:, b0 + 3:b0 + 4], accum_out=bis[:, b0 + 4:b0 + 5])
            else:
                TS('dve', MB.b[:, 0:W], SCv_[:, 0:W], bis[:, b0 + 3:b0 + 4], 0.0, ALU.is_gt, ALU.add,
                   SC_r_[0:nq] + [R['mid']], MB.all + [R['cnt']], accum_out=bis[:, b0 + 4:b0 + 5])

        def bis_update(i, slot, it):
            W = (i + 1) * 128
            b0 = slot * BW
            R = bR[slot]
            thr_cnt = float(512 - W) if slot == 0 else 256.0
            if it < NBIS - 1:
                TS('dve', bis[:, b0 + 5:b0 + 6], bis[:, b0 + 4:b0 + 5], thr_cnt, 0.5, ALU.is_lt, ALU.subtract,
                   [R['cnt']], [R['tmp']])
                STT(bis[:, b0 + 3:b0 + 4], bis[:, b0 + 5:b0 + 6], bis[:, b0 + 8 + it:b0 + 9 + it], bis[:, b0 + 3:b0 + 4],
                    ALU.mult, ALU.add, [R['tmp'], R['stp'], R['mid']], [R['mid']])
            else:
                TS('dve', bis[:, b0 + 5:b0 + 6], bis[:, b0 + 4:b0 + 5], thr_cnt, None, ALU.is_lt, None, [R['cnt']], [R['tmp']])
                if slot == 0:
                    STT(bis[:, b0 + 6:b0 + 7], bis[:, b0 + 5:b0 + 6], bis[:, b0 + 8 + it:b0 + 9 + it], bis[:, b0 + 3:b0 + 4],
                        ALU.mult, ALU.add, [R['tmp'], R['stp'], R['mid']], [R['tmp']])
                    TS('dve', bis[:, b0 + 7:b0 + 8], bis[:, b0 + 6:b0 + 7], -1.0, None, ALU.mult, None, [R['tmp']], [R['thr']])
                else:
                    STT(bis[:, b0 + 7:b0 + 8], bis[:, b0 + 5:b0 + 6], bis[:, b0 + 8 + it:b0 + 9 + it], bis[:, b0 + 3:b0 + 4],
                        ALU.mult, ALU.add, [R['tmp'], R['stp'], R['mid']], [R['thr']])

        def mask(i, slot, MB):
            W = (i + 1) * 128
            nq = (W + 255) // 256
            SCv_, SC_r_ = SCs[slot]
            b0 = slot * BW
            TS('dve', MB.b[:, 0:W], SCv_[:, 0:W], bis[:, b0 + 7:b0 + 8], NEGM, ALU.is_lt, ALU.mult,
               SC_r_[0:nq] + [bR[slot]['thr']], MB.q[0:(W + 511) // 512])

        def chain(gens):
            for g_ in gens:
                if g_ is not None:
                    yield from g_

        prev = None
        for p in range(8):
            ia, ib = 2 * p, 2 * p + 1
            MBa, MBb = MBs[(p % 2) * 2], MBs[(p % 2) * 2 + 1]
            scores(ia, 1, prev)
            scores(ib, 0, prev)
            if p >= 1:
                bis_init(ia, 1)
                bis_init(ib, 0)
                for it in range(NBIS):
                    bis_pass(ib, 0, MBb)
                    bis_pass(ia, 1, MBa)
                    bis_update(ib, 0, it)
                    bis_update(ia, 1, it)
                    if prev is not None:
                        next(prev, None)
            else:
                MEMSET('dve', bis[:, 7:8], -1e29, [bR[0]['thr']])
                MEMSET('dve', bis[:, BW + 7:BW + 8], -1e29, [bR[1]['thr']])
            mask(ia, 1, MBa)
            mask(ib, 0, MBb)
            if prev is not None:
                for _ in prev:
                    pass
            prev = chain([stage2(ia, MBa), stage2(ib, MBb)])
        for _ in prev:
            pass
        for h in range(4):
            dump(f'oaT{h}', OA[h].b, OA[h].all)
        guo(l, 'A', OA, [12, 14, 16, 18, 20], [(pages[0], pages[1]), (pages[2], pages[3])], pages[4])
    def branch_B(l):
        QTL, KTL, KHL, KHT, VTp, GT, OBraw, SM, RBp = (pages[7], pages[8], pages[9], pages[10], pages[11], pages[16],
                                                       pages[17], pages[18], pages[19])
        OB = [pages[12 + h] for h in range(4)]
        KHTv = KHT.b.rearrange("p (i d) -> p i d", i=16)
        VTv = VTp.b.rearrange("p (i d) -> p i d", i=16)

        resetm = SM.b[:, 1536:2048]
        resetm_r = SM.q[3]
        MEMSET('dve', resetm, 1.0, [resetm_r])
        MEMSET('dve', resetm.rearrange("p (c t) -> p c t", t=64)[:, :, 0:1], 0.0, [resetm_r])

        def half(pg, g):
            hf = g % 2
            return pg.f[:, hf * 512:(hf + 1) * 512], [pg.q[2 * hf], pg.q[2 * hf + 1]]

        for h in range(4):
            col = l * 4 + h
            o = OFF
            W, W_r = wload(20 + 2 * ((h + 1) % 2), 8, 512,
                           [(0, 128, w_in_d[l, :, o['bq'] + h * 128:o['bq'] + (h + 1) * 128]),
                            (128, 128, w_in_d[l, :, o['bf'] + h * 128:o['bf'] + (h + 1) * 128]),
                            (256, 128, w_in_d[l, :, o['bi'] + h * 128:o['bi'] + (h + 1) * 128]),
                            (384, 128, w_in_d[l, :, o['bg'] + h * 128:o['bg'] + (h + 1) * 128])])
            for g in range(NG):
                gs = slice(g * 512, (g + 1) * 512)
                E, E_r = half(pages[0], g)
                Fv, F_r = half(pages[1], g)
                Bc, B_r = half(pages[2], g)
                EB, EB_r = half(pages[3], g)
                ENB, ENB_r = half(pages[4], g)
                K32, K32_r = half(pages[5], g)
                KIN, KIN_r = half(pages[6], g)
                pf, pfr = psum('mmw', [0, 1, 2, 3, 6, 7])
                for c in range(NCH):
                    MM(pf, W[:, c, 128:256], HT[:, c, gs], c == 0, c == NCH - 1, W_r + [HTr[c][g]], [pfr])
                ACTV(E, pf, AF.Exp, [pfr], E_r, scale=-1.0)
                ACTV(E, E, AF.Ln, E_r, E_r, bias=1.0)
                ACTV(E, E, AF.Exp, E_r, E_r, scale=-1.0)
                ACTV(Fv, E, AF.Ln, E_r + [OMLB_r, LB_r], F_r, scale=OMLB[:, col:col + 1], bias=LB[:, col:col + 1])
                TS('dve', KIN, E, NOMLB[:, col:col + 1], OMLB[:, col:col + 1], ALU.mult, ALU.add,
                   E_r + [NOMLB_r, OMLB_r], KIN_r)
                k.op('dve', lambda E_, Bc=Bc, Fv=Fv: E_.tensor_tensor_scan(out=Bc, data0=resetm, data1=Fv, initial=0.0,
                                                                          op0=ALU.mult, op1=ALU.add),
                     reads=F_r + [resetm_r], writes=B_r)
                ACTV(DL[:, g * 8:(g + 1) * 8], Bc.rearrange("p (c t) -> p c t", t=64)[:, :, 63], AF.Exp, B_r, [DL_r])
                ACTV(EB, Bc, AF.Exp, B_r, EB_r)
                ACTV(ENB, Bc, AF.Exp, B_r, ENB_r, scale=-1.0)
                pq, pqr = psum('mmw', [0, 1, 2, 3, 6, 7])
                for c in range(NCH):
                    MM(pq, W[:, c, 0:128], HT[:, c, gs], c == 0, c == NCH - 1, W_r + [HTr[c][g]], [pqr])
                STT(QTL.b[:, gs], pq, B_SCALE, EB, ALU.mult, ALU.mult, [pqr] + EB_r, [QTL.q[g]])
                TT('dve', K32, KIN, ENB, ALU.mult, KIN_r + ENB_r, K32_r)
                CP('act', KTL.b[:, gs], K32, K32_r, [KTL.q[g]])
                TT('dve', KHL.b[:, gs].rearrange("p (c t) -> p c t", t=64), K32.rearrange("p (c t) -> p c t", t=64),
                   DL[:, g * 8:(g + 1) * 8].unsqueeze(2).to_broadcast([128, 8, 64]), ALU.mult, K32_r + [DL_r], [KHL.q[g]])
                pv, pvr = psum('mmw', [0, 1, 2, 3, 6, 7])
                for ti in range(4):
                    i = g * 4 + ti
                    for c in range(NCH):
                        MM(pv[:, ti * 128:(ti + 1) * 128], HT[:, c, i * 128:(i + 1) * 128], W[:, c, 256:384], c == 0,
                           c == NCH - 1, W_r + [HTr[c][g]], [pvr])
                CP('act', VTv[:, g * 4:(g + 1) * 4, :], pv.rearrange("p (i d) -> p i d", i=4), [pvr], [VTp.q[g]])
                pt_, ptr_ = psum('mmw', [0, 1, 2, 3, 6, 7])
                ptb = pt_.bitcast(BF16)
                for ti in range(4):
                    i = g * 4 + ti
                    TR(ptb[:, ti * 128:(ti + 1) * 128], KHL.b[:, i * 128:(i + 1) * 128], identb[:], [KHL.q[g], identb_r], [ptr_])
                CP('dve', KHTv[:, g * 4:(g + 1) * 4, :], ptb[:, 0:512].rearrange("p (i d) -> p i d", i=4), [ptr_], [KHT.q[g]])
            for g in range(NG):
                gs = slice(g * 512, (g + 1) * 512)
                pg_, pgr = psum('mmw', [0, 1, 2, 3, 6, 7])
                for c in range(NCH):
                    MM(pg_, W[:, c, 384:512], HT[:, c, gs], c == 0, c == NCH - 1, W_r + [HTr[c][g]], [pgr])
                ACTV(GT.b[:, gs], pg_, AF.Silu, [pgr], [GT.q[g]])
            atm_res = [Res(f'atm{j}') for j in range(4)]
            MEMSET('dve', S32s[0][0][:], 0.0, [S32s[0][1], SM.q[0]])
            MEMSET('dve', SBF[0][0][:], 0.0, [SBF[0][1]])
            OTp = OTr = None
            for c in range(32):
                i, hf, g = c // 2, c % 2, c // 8
                po = hf * 64
                cs = slice(c * 64, (c + 1) * 64)
                ap_, apr = psum('st', [2, 3])
                MM(ap_[po:po + 64, 0:64], KTL.b[:, cs], QTL.b[:, cs], True, True, [KTL.q[g], QTL.q[g]], [apr])
                up_, upr = psum('rs', [6, 7])
                MM(up_[:, 0:128], KHTv[po:po + 64, i, :], VTv[po:po + 64, i, :], True, True, [KHT.q[g], VTp.q[g]], [upr])
                ao = ((c // 2) % 2) * 64
                atm = SM.b[po:po + 64, ao:ao + 64]
                atm_r = atm_res[(c % 2) * 2 + (c // 2) % 2]
                TT('dve', atm, ap_[po:po + 64, 0:64], caus01[po:po + 64, :], ALU.mult, [apr, caus01_r, SM.q[0]], [atm_r])
                s_old, s_old_r = S32s[c % 2]
                s_new, s_new_r = S32s[(c + 1) % 2]
                STT(s_new[:], s_old[:], DL[:, c:c + 1], up_[:, 0:128], ALU.mult, ALU.add, [s_old_r, DL_r, upr], [s_new_r])
                nb_, nb_r = SBF[(c + 1) % 2]
                CP('act', nb_[:], s_new[:], [s_new_r], [nb_r])
                if c % 8 == 0:
                    OTp, OTr = psum('ot', [4, 5])
                sl = slice((c % 8) * 64, (c % 8) * 64 + 64)
                sbf, sbf_r = SBF[c % 2]
                MM(OTp[:, sl], sbf[:], QTL.b[:, cs], True, False, [sbf_r, QTL.q[g]], [OTr])
                MM(OTp[:, sl], VTv[po:po + 64, i, :], atm, False, True, [VTp.q[g], atm_r], [OTr])
                if c % 8 == 7:
                    CP('act', OBraw.b[:, g * 512:(g + 1) * 512], OTp, [OTr], [OBraw.q[g]])
            MEMSET('dve', DL[:, 0:1], 0.0, atm_res + [SM.q[0], DL_r])
            for g in range(NG):
                gs = slice(g * 512, (g + 1) * 512)
                rb, rb_res = half(RBp, g)
                pn, pnr = psum('nrm', [6, 7])
                ACTV(SM.b[:, 512:1024], OBraw.b[:, gs], AF.Square, [OBraw.q[g]], [SM.q[1]])
                MM(pn, onesb[:], SM.b[:, 512:1024], True, True, [onesb_r, SM.q[1]], [pnr])
                ACTV(rb, pn, AF.Ln, [pnr], rb_res, scale=1.0 / 128, bias=EPS)
                ACTV(rb, rb, AF.Exp, rb_res, rb_res, scale=-0.5)
                STT(SM.b[:, 1024:1536], OBraw.b[:, gs], gvec[:, VC_HGN(l):VC_HGN(l) + 1], rb, ALU.mult, ALU.mult,
                    [OBraw.q[g], gvec_r] + rb_res, [SM.q[2]])
                TT('dve', OB[h].b[:, gs], SM.b[:, 1024:1536], GT.b[:, gs], ALU.mult, [SM.q[2], GT.q[g]], [OB[h].q[g]])
        for h in range(4):
            dump(f'obT{h}', OB[h].b, OB[h].all)
        guo(l, 'B', OB, [0, 2, 4, 16, 18], [(pages[6], pages[7]), (pages[8], pages[9])], pages[10])

    def ffn(l):
        norm_to_HT(VC_FFN(l), 2, 4)
        hid = [pages[i] for i in (0, 1, 5, 6, 7, 8, 9, 10)]
        tmp = pages[11]
        slots = [12, 18, 14, 16, 20, 22]
        sr = [0]

        def nslot():
            s_ = slots[sr[0] % len(slots)]
            sr[0] += 1
            return s_
        f0 = 0
        while f0 < NFF:
            nf = min(8, NFF - f0)
            downs = []
            for s0 in range(0, nf, 4):
                ns = min(4, nf - s0)
                fa = f0 + s0
                wg, wg_r = wload(nslot(), 8, ns * 128, [(0, ns * 128, wg_d[l, :, fa * 128:(fa + ns) * 128])])
                wu, wu_r = wload(nslot(), 8, ns * 128, [(0, ns * 128, wu_d[l, :, fa * 128:(fa + ns) * 128])])
                wdn, wdn_r = wload(nslot(), ns, 1024, [(0, 1024, wd_d[l, fa * 128:(fa + ns) * 128, :])])
                downs.append((s0, ns, wdn, wdn_r))
                for j in range(ns):
                    hp = hid[s0 + j]
                    for g in range(NG):
                        gs = slice(g * 512, (g + 1) * 512)
                        pg_, pgr = psum('ffg', [0, 1])
                        for c in range(NCH):
                            MM(pg_, wg[:, c, j * 128:(j + 1) * 128], HT[:, c, gs], c == 0, c == NCH - 1, wg_r + [HTr[c][g]], [pgr])
                        pu_, pur = psum('ffu', [2, 3])
                        for c in range(NCH):
                            MM(pu_, wu[:, c, j * 128:(j + 1) * 128], HT[:, c, gs], c == 0, c == NCH - 1, wu_r + [HTr[c][g]], [pur])
                        tq = g % 4
                        ACTV(tmp.b[:, tq * 512:(tq + 1) * 512], pg_, AF.Silu, [pgr], [tmp.q[tq]])
                        TT('dve', hp.b[:, gs], tmp.b[:, tq * 512:(tq + 1) * 512], pu_, ALU.mult, [tmp.q[tq], pur], [hp.q[g]])
            for n in range(NCH):
                for g in range(NG):
                    gs = slice(g * 512, (g + 1) * 512)
                    py, pyr = psum('ffy', [4, 5])
                    first = True
                    for (s0, ns, wdn, wdn_r) in downs:
                        for j in range(ns):
                            hp = hid[s0 + j]
                            MM(py, wdn[:, j, n * 128:(n + 1) * 128], hp.b[:, gs], first, s0 + j == nf - 1, wdn_r + [hp.q[g]], [pyr])
                            first = False
                    TT('dve', XT[:, n, gs], XT[:, n, gs], py, ALU.add, [XTr[n][g], pyr], [XTr[n][g]])
            f0 += nf

    for l in range(layer0, layer0 + n_layers):
        if any(p in parts for p in 'ABC'):
            norm_to_HT(VC_ATTN(l), 22, 21)
        if 'A' in parts:
            branch_A(l)
        if 'B' in parts:
            branch_B(l)
        if 'C' in parts:
            branch_C(l)
        if 'ffn' in parts:
            ffn(l)
        if l == 0:
            for c in range(NCH):
                dump(f'xl0_{c}', XT[:, c, :], XTr[c])

    if 'final' in parts:
        for g in range(NG):
            rb = pages[4]
            rap = rb.f[:, 0:512]
            rres = [rb.q[0], rb.q[1]]
            rbc_from(lambda c: XT[:, c, g * 512:(g + 1) * 512], lambda c: [XTr[c][g]], NCH, 1.0 / D, g,
                     [pages[2], pages[3]], rap, rres)
            for c in range(NCH):
                STT(XT[:, c, g * 512:(g + 1) * 512], XT[:, c, g * 512:(g + 1) * 512], gvec[:, VC_FINAL + c:VC_FINAL + c + 1],
                    rap, ALU.mult, ALU.mult, [XTr[c][g], gvec_r] + rres, [XTr[c][g]])
    for i in range(16):
        g = i // 4
        og = pages[6 + (i % 2)]
        for half_ in range(2):
            pap, pr = psum('tr', [0, 1])
            for j in range(4):
                c = half_ * 4 + j
                TR(pap[:, j * 128:(j + 1) * 128], XT[:, c, i * 128:(i + 1) * 128], identf[:], [XTr[c][g], identf_r], [pr])
            CP('dve' if half_ == 0 else 'act', og.f[:, half_ * 512:(half_ + 1) * 512], pap, [pr],
               [og.q[2 * half_], og.q[2 * half_ + 1]])
        DMA('sp', out_d[i * 128:(i + 1) * 128, :], og.f, reads=og.all, is_out=True)

    k.wait_tokens('sp', k.out_tokens)
    stats = k.finish()
    return nc, es, stats


def const_inputs():
    t = np.arange(128)
    c = {}
    c["c_identf"] = np.eye(128, dtype=np.float32)
    c["c_cmaskf"] = np.where(t[None, :] <= t[:, None], 0.0, -1e30).astype(np.float32)
    c["c_cmaskb"] = np.where(t[None, :] <= t[:, None], 0.0, NEGM).astype(np.float32)
    s64 = np.arange(128) % 64
    c["c_caus01"] = (s64[:, None] <= np.arange(64)[None, :]).astype(np.float32)
    def bucket(d):
        d = np.maximum(d, 0)
        dl = np.maximum(d, 16).astype(np.float32)
        large = 16 + (np.log(dl / np.float32(16)) / np.float32(np.log(128 / 16)) * np.float32(16)).astype(np.int32)
        large = np.minimum(large, 31)
        return np.where(d < 16, d, large)
    oh = np.zeros((128, 64, 128), np.float32)
    for tbl in range(2):
        d = (t[:, None] - t[None, :]) + 128 * tbl
        bk = bucket(d)
        for b in range(32):
            oh[:, tbl * 32 + b, :] = (bk == b)
    c["c_onehot"] = oh.reshape(128, 64 * 128)
    c["c_pow2"] = np.tile((2.0 ** -(np.arange(NBIS) + 1.0)).astype(np.float32)[None, :], (128, 1))
    inv_freq = (10000.0 ** (-np.arange(0, 64, 2, dtype=np.float32) / np.float32(64))).astype(np.float32)
    rope = np.zeros((64, 2), np.float32)
    rope[:, 0] = np.concatenate([inv_freq, inv_freq])
    rope[:, 1] = np.concatenate([-np.ones(32), np.ones(32)])
    c["c_rope"] = rope
    return c


W_NAMES = ("w_in", "w_up_a", "w_up_b", "w_up_c", "w_out", "mla_w_qb", "mla_w_kvb", "w_ffn_gate", "w_ffn_up", "w_ffn_down")


def core_inputs(inputs, b, consts, vecs, relb):
    m = dict(consts)
    m["x"] = np.ascontiguousarray(inputs["x"][b])
    m["pos64"] = np.ascontiguousarray(np.broadcast_to(inputs["positions"][b][None, :], (64, T))).astype(np.int32)
    m["vecs"] = vecs
    m["relb"] = relb
    for wn in W_NAMES:
        m[wn] = inputs[wn]
    return m


def kernel(**inputs):
    inputs = {k_: np.asarray(v) for k_, v in inputs.items()}
    nc, es, stats = build(parts=('A', 'B', 'C', 'ffn', 'final'))
    consts = const_inputs()
    vecs = make_vecs(inputs)
    relb = np.ascontiguousarray(np.broadcast_to(inputs["rel_bias"].reshape(1, 128), (128, 128))).astype(np.float32)
    for wn in W_NAMES:
        inputs[wn] = np.ascontiguousarray(inputs[wn], dtype=np.float32)
    in_maps = [core_inputs(inputs, b, consts, vecs, relb) for b in range(8)]
    res = run_bass_kernel_spmd(nc, in_maps, core_ids=list(range(8)))
    es.close()
    return np.stack([np.asarray(r["out"], dtype=np.float32) for r in res.results], axis=0)
```

```python
import numpy as np
import concourse.bass as bass
import concourse.mybir as mybir
from concourse.bass_utils import run_bass_kernel_spmd
from contextlib import ExitStack

F32 = mybir.dt.float32
BF16 = mybir.dt.bfloat16
I32 = mybir.dt.int32
ALU = mybir.AluOpType
AF = mybir.ActivationFunctionType
AX = mybir.AxisListType

ENGS = ['pe', 'act', 'dve', 'pool', 'sp']
EPOCH = 30000
NDSEM = 24
NPSEM = 12
STRICT = True

T = 2048
D = 1024
NCH = 8
NG = 4
DEPTH = 4
DFF = 2816
NFF = 22
EPS = 1e-6
IN_COLS = 7176
OFF = dict(aq=0, ak=512, av=640, iq=768, ik=1280, iw=1344, bq=1352, bf=1864, bi=2376, bg=2888,
           cq=3400, ckv=3784, ckpe=4040, ga=4104, gb=5128, gc=6152)


VC_FINAL = 0
def VC_ATTN(l): return 8 + 22 * l
def VC_FFN(l): return 8 + 22 * l + 8
def VC_QN(l): return 8 + 22 * l + 16
def VC_KVN(l): return 8 + 22 * l + 19
def VC_HGN(l): return 8 + 22 * l + 21
VC_LB = 8 + 22 * DEPTH
NV = VC_LB + 16


def make_vecs(inp):
    v = np.zeros((128, NV), np.float32)
    v[:, 0:8] = inp["final_norm"].reshape(8, 128).T
    for l in range(DEPTH):
        v[:, VC_ATTN(l):VC_ATTN(l) + 8] = inp["attn_norm"][l].reshape(8, 128).T
        v[:, VC_FFN(l):VC_FFN(l) + 8] = inp["ffn_norm"][l].reshape(8, 128).T
        v[:, VC_QN(l):VC_QN(l) + 3] = inp["mla_q_norm"][l].reshape(3, 128).T
        v[:, VC_KVN(l):VC_KVN(l) + 2] = inp["mla_kv_norm"][l].reshape(2, 128).T
        v[:, VC_HGN(l)] = inp["hgrn_out_norm"][l]
        v[:, VC_LB + 4 * l:VC_LB + 4 * l + 4] = inp["hgrn_lb_logits"][l].reshape(4, 128).T
    return v


class Res:
    __slots__ = ('name', 'excl', 'w', 'r')

    def __init__(self, name, excl=False):
        self.name = name
        self.excl = excl
        self.w = None
        self.r = {}


class KB:
    def __init__(self, nc, es):
        self.nc = nc
        self.es = es
        self.e = {'pe': nc.tensor, 'act': nc.scalar, 'dve': nc.vector, 'pool': nc.gpsimd, 'sp': nc.sync}
        self.ops = []
        self.cnt = {e: 0 for e in ENGS}
        self.known = {e: {} for e in ENGS}
        self.sig = {e: set() for e in ENGS}
        self.dval = [0] * (NDSEM + NPSEM)
        self.dnext = 0
        self.pnext = 0
        self.out_tokens = []

    def _need(self, e, tok, waits):
        key, val = tok
        if self.known[e].get(key, 0) >= val:
            return
        self.known[e][key] = val
        waits.append(tok)
        if isinstance(key, str):
            self.sig[key].add(val)

    def op(self, eng, fn, reads=(), writes=(), dma=False, is_out=False):
        waits = []
        self.cnt[eng] += 1
        idx = self.cnt[eng]
        if dma:
            if eng == 'pool':
                s = NDSEM + self.pnext
                self.pnext = (self.pnext + 1) % NPSEM
            else:
                s = self.dnext
                self.dnext = (s + 1) % NDSEM
            prev = self.dval[s]
            if prev > 0:
                self._need(eng, (('d', s), prev), waits)
            self.dval[s] = prev + 16
            mytok = (('d', s), prev + 16)
        else:
            mytok = (eng, idx)
        for r in reads:
            if r.w is not None:
                k_ = r.w[0]
                if dma or k_ != eng or eng != 'pe':
                    self._need(eng, r.w, waits)
            if r.excl:
                for k_, v_ in r.r.items():
                    if dma or k_ != eng:
                        self._need(eng, (k_, v_), waits)
        strict = STRICT and eng != 'pe'
        for r in writes:
            if r.w is not None:
                k_ = r.w[0]
                if dma or k_ != eng or strict:
                    self._need(eng, r.w, waits)
            for k_, v_ in r.r.items():
                if dma or k_ != eng or strict:
                    self._need(eng, (k_, v_), waits)
        for r in reads:
            if r.excl:
                r.w = mytok
                r.r = {}
            else:
                r.r[mytok[0]] = mytok[1]
        for r in writes:
            r.w = mytok
            r.r = {}
        self.ops.append((eng, idx, fn, waits, mytok if dma else None))
        if is_out:
            self.out_tokens.append(mytok)
        return mytok

    def wait_tokens(self, eng, toks):
        waits = []
        for t in toks:
            self._need(eng, t, waits)
        self.cnt[eng] += 1
        self.ops.append((eng, self.cnt[eng], None, waits, None))

    def finish(self):
        nc = self.nc
        rank = {}
        esem = {}
        for e in ENGS:
            lst = sorted(self.sig[e])
            rank[e] = {v: i + 1 for i, v in enumerate(lst)}
            n = max(1, (len(lst) + EPOCH - 1) // EPOCH)
            esem[e] = [self.es.enter_context(nc.semaphore(f"s_{e}{i}")) for i in range(n)]
        dsem = [self.es.enter_context(nc.semaphore(f"d{i}")) for i in range(NDSEM + NPSEM)]
        nwait = 0
        for (eng, idx, fn, waits, dtok) in self.ops:
            E = self.e[eng]
            for key, val in waits:
                nwait += 1
                if isinstance(key, str):
                    rk = rank[key][val]
                    E.wait_ge(esem[key][(rk - 1) // EPOCH], (rk - 1) % EPOCH + 1)
                else:
                    E.wait_ge(dsem[key[1]], val)
            if fn is None:
                continue
            inst = fn(E)
            if dtok is not None:
                inst.then_inc(dsem[dtok[0][1]], 16)
            elif idx in rank[eng]:
                rk = rank[eng][idx]
                inst.then_inc(esem[eng][(rk - 1) // EPOCH], 1)
        self.stats = dict(cnt=dict(self.cnt), nwait=nwait, nsig={e: len(self.sig[e]) for e in ENGS})
        return self.stats


class Page:
    def __init__(self, ap32, name):
        self.f = ap32
        self.b = ap32.bitcast(BF16)
        self.q = [Res(f"{name}.{i}") for i in range(4)]

    @property
    def all(self):
        return self.q


A_SCALE = 128 ** -0.5
C_SCALE = 192 ** -0.5
B_SCALE = 128 ** -0.5
TWO_PI_HI = 6.28125
TWO_PI_LO = 2.0 * np.pi - 6.28125
NBIS = 11
NEGM = -30000.0


def build(n_layers=DEPTH, dbg=None, parts=('A', 'B', 'C', 'ffn'), layer0=0):
    dbg = dbg or {}
    nc = bass.Bass("TRN2", target_bir_lowering=False)
    es = ExitStack()
    k = KB(nc, es)

    def dr(name, shape, dt=F32, kind="ExternalInput"):
        return nc.dram_tensor(name, list(shape), dt, kind=kind).ap()

    x_d = dr("x", [T, D])
    out_d = dr("out", [T, D], kind="ExternalOutput")
    pos_d = dr("pos64", [64, T], I32)
    vecs_d = dr("vecs", [128, NV])
    relb_d = dr("relb", [128, 128])
    w_in_d = dr("w_in", [DEPTH, D, IN_COLS])
    w_up_d = {'A': dr("w_up_a", [DEPTH, 512, D]), 'B': dr("w_up_b", [DEPTH, 512, D]), 'C': dr("w_up_c", [DEPTH, 512, D])}
    w_out_d = dr("w_out", [DEPTH, D, D])
    w_qb_d = dr("mla_w_qb", [DEPTH, 384, 768])
    w_kvb_d = dr("mla_w_kvb", [DEPTH, 256, 1024])
    wg_d = dr("w_ffn_gate", [DEPTH, D, DFF])
    wu_d = dr("w_ffn_up", [DEPTH, D, DFF])
    wd_d = dr("w_ffn_down", [DEPTH, DFF, D])
    identf_d = dr("c_identf", [128, 128])
    cmaskf_d = dr("c_cmaskf", [128, 128])
    cmaskb_d = dr("c_cmaskb", [128, 128])
    caus01_d = dr("c_caus01", [128, 64])
    onehot_d = dr("c_onehot", [128, 64 * 128])
    pow2_d = dr("c_pow2", [128, NBIS])
    rope_d = dr("c_rope", [64, 2])
    dump_d = {n: dr("dbg_" + n, shp[0], shp[1], kind="ExternalOutput") for n, shp in dbg.items()}

    def sb(name, shape, dt=F32):
        return es.enter_context(nc.sbuf_tensor(name, list(shape), dt))

    XT = sb("XT", [128, NCH, T])
    XTr = [[Res(f"XT{c}.{g}") for g in range(NG)] for c in range(NCH)]
    HT = sb("HT", [128, NCH, T], BF16)
    HTr = [[Res(f"HT{c}.{g}") for g in range(NG)] for c in range(NCH)]
    NPG = 24
    GP = sb("GP", [128, NPG, 1024])
    pages = [Page(GP[:, i, :], f"pg{i}") for i in range(NPG)]
    PS = es.enter_context(nc.psum_tensor("PS", [128, 8, 512], F32))
    PSr = [Res(f"ps{i}", excl=True) for i in range(8)]
    ps_rr = {}

    def psum(tag, banks):
        i = ps_rr.get(tag, 0)
        ps_rr[tag] = i + 1
        b = banks[i % len(banks)]
        return PS[:, b, :], PSr[b]

    def small(name, shape, dt=F32):
        return sb("s_" + name, shape, dt), Res(name)

    identf, identf_r = small("identf", [128, 128])
    identb, identb_r = small("identb", [128, 128], BF16)
    onesb, onesb_r = small("onesb", [128, 128], BF16)
    gvec, gvec_r = small("gvec", [128, NV])
    cmaskf, cmaskf_r = small("cmaskf", [128, 128])
    cmaskb, cmaskb_r = small("cmaskb", [128, 128], BF16)
    caus01, caus01_r = small("caus01", [128, 64], BF16)
    pow2, pow2_r = small("pow2", [128, NBIS])
    ropec, ropec_r = small("ropec", [64, 2])
    biasT, biasT_r = small("biasT", [128, 8, 128], BF16)
    cosT, cosT_r = small("cosT", [64, T], BF16)
    sinT, sinT_r = small("sinT", [64, T], BF16)
    LB, LB_r = small("LB", [128, 16])
    OMLB, OMLB_r = small("OMLB", [128, 16])
    NOMLB, NOMLB_r = small("NOMLB", [128, 16])
    IW, IW_r = small("IW", [128, 16, 8])
    DL, DL_r = small("DL", [128, 32])
    S32s = [small(f"S32{i}", [128, 128]) for i in range(2)]
    SBF = [small(f"SBF{i}", [128, 128], BF16) for i in range(2)]
    bis, bis_r = small("bis", [128, 64])

    def DMA(q, out, in_, reads=(), writes=(), is_out=False):
        return k.op(q, lambda E: E.dma_start(out=out, in_=in_), reads=reads, writes=writes, dma=True, is_out=is_out)

    def MM(out, lhsT, rhs, start, stop, reads, writes, skip=False):
        k.op('pe', lambda E: E.matmul(out=out, lhsT=lhsT, rhs=rhs, start=start, stop=stop, skip_group_check=skip),
             reads=reads, writes=writes)

    def TR(out, in_, ident, reads, writes):
        k.op('pe', lambda E: E.transpose(out=out, in_=in_, identity=ident), reads=reads, writes=writes)

    def ACTV(out, in_, func, reads, writes, scale=1.0, bias=None, accum_out=None):
        kw = {}
        if bias is not None:
            kw['bias'] = bias
        if accum_out is not None:
            kw['accum_out'] = accum_out
        k.op('act', lambda E: E.activation(out=out, in_=in_, func=func, scale=scale, **kw), reads=reads, writes=writes)

    def TT(eng, out, in0, in1, op, reads, writes):
        k.op(eng, lambda E: E.tensor_tensor(out=out, in0=in0, in1=in1, op=op), reads=reads, writes=writes)

    def TS(eng, out, in0, s1, s2, op0, op1, reads, writes, accum_out=None):
        kw = {}
        if accum_out is not None:
            kw['accum_out'] = accum_out
        if op1 is None:
            k.op(eng, lambda E: E.tensor_scalar(out=out, in0=in0, scalar1=s1, scalar2=None, op0=op0, **kw),
                 reads=reads, writes=writes)
        else:
            k.op(eng, lambda E: E.tensor_scalar(out=out, in0=in0, scalar1=s1, scalar2=s2, op0=op0, op1=op1, **kw),
                 reads=reads, writes=writes)

    def STT(out, in0, scalar, in1, op0, op1, reads, writes):
        k.op('dve', lambda E: E.scalar_tensor_tensor(out=out, in0=in0, scalar=scalar, in1=in1, op0=op0, op1=op1),
             reads=reads, writes=writes)

    def CP(eng, out, in_, reads, writes):
        if eng == 'act':
            k.op('act', lambda E: E.activation(out=out, in_=in_, func=AF.Copy), reads=reads, writes=writes)
        else:
            k.op(eng, lambda E: E.tensor_copy(out=out, in_=in_), reads=reads, writes=writes)

    def RECIP(out, in_, reads, writes):
        k.op('dve', lambda E: E.reciprocal(out=out, in_=in_), reads=reads, writes=writes)

    def MEMSET(eng, ap, val, writes):
        k.op(eng, lambda E: E.memset(ap, val), writes=writes)

    def dump(name, ap, res):
        if name in dump_d:
            DMA('sp', dump_d[name], ap, reads=res, is_out=True)

    def slot(p0):
        base = GP[:, p0:p0 + 2, :].rearrange("p a b -> p (a b)").bitcast(BF16)
        return base, pages[p0].all + pages[p0 + 1].all

    def wload(p0, kc, ncols, parts_):
        assert kc * ncols <= 4096
        base, res = slot(p0)
        view = base[:, 0:kc * ncols].rearrange("p (k n) -> p k n", k=kc)
        for (c0, ncol, src) in parts_:
            DMA('pool', view[:, :, c0:c0 + ncol], src.rearrange("(k p) n -> p k n", p=128), writes=res)
        return view, res

    DMA('sp', identf[:], identf_d, writes=[identf_r])
    DMA('sp', gvec[:], vecs_d, writes=[gvec_r])
    DMA('sp', cmaskf[:], cmaskf_d, writes=[cmaskf_r])
    DMA('pool', cmaskb[:], cmaskb_d, writes=[cmaskb_r])
    DMA('pool', caus01[:], caus01_d, writes=[caus01_r])
    DMA('sp', pow2[:], pow2_d, writes=[pow2_r])
    DMA('sp', ropec[:], rope_d, writes=[ropec_r])
    CP('dve', identb[:], identf[:], [identf_r], [identb_r])
    MEMSET('dve', onesb[:], 1.0, [onesb_r])
    for i in range(16):
        pg = pages[20 + (i % 2)]
        DMA('sp' if i % 2 == 0 else 'act', pg.f, x_d[i * 128:(i + 1) * 128, :], writes=pg.all)
        g = i // 4
        for half in range(2):
            pap, pr = psum('tr', [0, 1])
            for j in range(4):
                c = half * 4 + j
                TR(pap[:, j * 128:(j + 1) * 128], pg.f[:, c * 128:(c + 1) * 128], identf[:], pg.all + [identf_r], [pr])
            CP('act', XT[:, half * 4:half * 4 + 4, i * 128:(i + 1) * 128],
               pap.rearrange("p (c t) -> p c t", c=4), [pr], [XTr[c][g] for c in range(half * 4, half * 4 + 4)])

    def pairf(p0):
        return GP[:, p0:p0 + 2, :].rearrange("p a b -> p (a b)"), pages[p0].all + pages[p0 + 1].all

    PIv, PI_r = pairf(0)
    Av, A_r = pairf(2)
    Nv, N_r = pairf(4)
    Rv, R_r = pairf(6)
    PIi = PIv.bitcast(I32)
    DMA('sp', PIi[0:64, :], pos_d, writes=PI_r)
    CP('dve', Av[0:64, :], PIi[0:64, :], PI_r, A_r)
    TS('dve', Av[0:64, :], Av[0:64, :], ropec[:, 0:1], None, ALU.mult, None, A_r + [ropec_r], A_r)
    for which in (0, 1):
        shift = 0.0 if which == 0 else 0.5 * np.pi
        TS('dve', Nv[0:64, :], Av[0:64, :], 1.0 / (2 * np.pi), shift / (2 * np.pi), ALU.mult, ALU.add, A_r, N_r)
        CP('dve', PIi[0:64, :], Nv[0:64, :], N_r, PI_r)
        CP('dve', Nv[0:64, :], PIi[0:64, :], PI_r, N_r)
        STT(Rv[0:64, :], Nv[0:64, :], -TWO_PI_HI, Av[0:64, :], ALU.mult, ALU.add, N_r + A_r, R_r)
        STT(Rv[0:64, :], Nv[0:64, :], -TWO_PI_LO, Rv[0:64, :], ALU.mult, ALU.add, N_r + R_r, R_r)
        if which == 1:
            TS('dve', Rv[0:64, :], Rv[0:64, :], shift, None, ALU.add, None, R_r, R_r)
        TS('dve', Rv[0:64, :], Rv[0:64, :], -3.1415925, 3.1415925, ALU.max, ALU.min, R_r, R_r)
        if which == 0:
            ACTV(Nv[0:64, :], Rv[0:64, :], AF.Sin, R_r, N_r)
            TS('dve', sinT[:], Nv[0:64, :], ropec[:, 1:2], None, ALU.mult, None, N_r + [ropec_r], [sinT_r])
        else:
            ACTV(cosT[:], Rv[0:64, :], AF.Sin, R_r, [cosT_r])
    dump('cosT', cosT[:], [cosT_r])
    dump('sinT', sinT[:], [sinT_r])

    OHv = GP[:, 8:12, :].rearrange("p a b -> p (a b)").bitcast(BF16).rearrange("p (n s) -> p n s", s=128)
    OH_r = pages[8].all + pages[9].all + pages[10].all + pages[11].all
    DMA('pool', OHv, onehot_d.rearrange("p (n s) -> p n s", s=128), writes=OH_r)
    relb = pages[13].f[:, 0:128]
    relb_r = pages[13].q[0]
    DMA('sp', relb, relb_d, writes=[relb_r])
    ACC = pages[12].f.rearrange("p (n s) -> p n s", s=128)
    ACC_r = pages[12].all
    accr = [Res(f"acc{j}") for j in range(8)]
    MEMSET('dve', pages[12].f, 0.0, ACC_r + accr)
    for b in range(32):
        for h in range(4):
            for tbl in range(2):
                hj = h * 2 + tbl
                STT(ACC[:, hj, :], OHv[:, tbl * 32 + b, :], relb[:, b * 4 + h:b * 4 + h + 1], ACC[:, hj, :],
                    ALU.mult, ALU.add, OH_r + [accr[hj], relb_r], [accr[hj]])
    for h in range(4):
        for tbl in range(2):
            hj = h * 2 + tbl
            TS('dve', biasT[:, hj, :], ACC[:, hj, :], relb[:, 31 * 4 + h:31 * 4 + h + 1], None, ALU.subtract, None,
               [accr[hj], relb_r], [biasT_r])
    MEMSET('dve', bis[:, 0:1], 0.0, ACC_r + accr + [bis_r])
    dump('biasT', biasT[:], [biasT_r])

    lbs, lbs_r = small("lbs", [128, 64])
    Lg = lambda l: gvec[:, VC_LB + 4 * l:VC_LB + 4 * l + 4]
    TT('dve', lbs[:, 0:4], Lg(0), Lg(1), ALU.max, [gvec_r], [lbs_r])
    TT('dve', lbs[:, 0:4], lbs[:, 0:4], Lg(2), ALU.max, [gvec_r, lbs_r], [lbs_r])
    TT('dve', lbs[:, 0:4], lbs[:, 0:4], Lg(3), ALU.max, [gvec_r, lbs_r], [lbs_r])
    for l in range(4):
        TT('dve', lbs[:, 8 + 4 * l:12 + 4 * l], Lg(l), lbs[:, 0:4], ALU.subtract, [gvec_r, lbs_r], [lbs_r])
    ACTV(lbs[:, 8:24], lbs[:, 8:24], AF.Exp, [lbs_r], [lbs_r])
    TT('dve', lbs[:, 4:8], lbs[:, 8:12], lbs[:, 12:16], ALU.add, [lbs_r], [lbs_r])
    TT('dve', lbs[:, 4:8], lbs[:, 4:8], lbs[:, 16:20], ALU.add, [lbs_r], [lbs_r])
    TT('dve', lbs[:, 4:8], lbs[:, 4:8], lbs[:, 20:24], ALU.add, [lbs_r], [lbs_r])
    RECIP(lbs[:, 4:8], lbs[:, 4:8], [lbs_r], [lbs_r])
    for l in range(4):
        TT('dve', lbs[:, 8 + 4 * l:12 + 4 * l], lbs[:, 8 + 4 * l:12 + 4 * l], lbs[:, 4:8], ALU.mult, [lbs_r], [lbs_r])
    MEMSET('dve', LB[:, 0:4], 0.0, [LB_r])
    CP('dve', LB[:, 4:8], lbs[:, 12:16], [lbs_r], [LB_r])
    TT('dve', LB[:, 8:12], LB[:, 4:8], lbs[:, 16:20], ALU.add, [lbs_r, LB_r], [LB_r])
    TT('dve', LB[:, 12:16], LB[:, 8:12], lbs[:, 20:24], ALU.add, [lbs_r, LB_r], [LB_r])
    TS('dve', OMLB[:], LB[:], -1.0, 1.0, ALU.mult, ALU.add, [LB_r], [OMLB_r])
    TS('dve', NOMLB[:], LB[:], 1.0, None, ALU.subtract, None, [LB_r], [NOMLB_r])
    dump('LB', LB[:], [LB_r])

    def rbc_from(sq_src, src_res, nchunks, inv_n, g, sq_pgs, rbc_ap, rbc_res):
        pap, pr = psum('nrm', [6, 7])
        for c in range(nchunks):
            sp = sq_pgs[c % len(sq_pgs)]
            qi = (c // len(sq_pgs)) % 4
            ACTV(sp.b[:, qi * 512:(qi + 1) * 512], sq_src(c), AF.Square, src_res(c), [sp.q[qi]])
            MM(pap, onesb[:], sp.b[:, qi * 512:(qi + 1) * 512], c == 0, c == nchunks - 1, [sp.q[qi], onesb_r], [pr])
        ACTV(rbc_ap, pap, AF.Ln, [pr], rbc_res, scale=inv_n, bias=EPS)
        ACTV(rbc_ap, rbc_ap, AF.Exp, rbc_res, rbc_res, scale=-0.5)

    def norm_to_HT(gcol, sqp, rbp):
        for g in range(NG):
            rb = pages[rbp]
            hq = (g % 2) * 2
            rap = rb.f[:, (g % 2) * 512:(g % 2) * 512 + 512]
            rres = [rb.q[hq], rb.q[hq + 1]]
            rbc_from(lambda c: XT[:, c, g * 512:(g + 1) * 512], lambda c: [XTr[c][g]], NCH, 1.0 / D, g,
                     [pages[sqp], pages[sqp + 1]], rap, rres)
            for c in range(NCH):
                STT(HT[:, c, g * 512:(g + 1) * 512], XT[:, c, g * 512:(g + 1) * 512], gvec[:, gcol + c:gcol + c + 1], rap,
                    ALU.mult, ALU.mult, [XTr[c][g], gvec_r] + rres, [HTr[c][g]])
    at_rr = [0]

    def attn_group(i0, nqb, wide_terms, blk_terms, v_of_j, out_ap, out_res, pt_bufs, rs_bufs, rs_in_ot=False):
        NQ = nqb * 128
        jlast = i0 + nqb - 1
        per_bank = 4 // nqb
        OTp, OTr = psum('ot', [4, 5])
        if rs_in_ot:
            assert NQ <= 256
            RSp, RSr = OTp[:, 256:512], OTr
        else:
            RSp, RSr = psum('rs', [6, 7])
        chunks = []
        j = 0
        while j <= jlast:
            nj = min(per_bank, jlast - j + 1)
            chunks.append((j, nj))
            j += nj

        def stage_qk(j, nj):
            STp, STr = psum('st', [2, 3])
            pt, ptr = pt_bufs[at_rr[0] % len(pt_bufs)]
            at_rr[0] += 1
            spans = []
            for jj in range(nj):
                jk = j + jj
                ia = max(i0, jk)
                off = (ia - i0) * 128
                wd = NQ - off
                base = jj * NQ
                tl = [(l_ap, l_res, r_ap[:, off:NQ], r_res, base + off, wd) for (l_ap, l_res, r_ap, r_res) in wide_terms(jk)]
                for i in range(ia, i0 + nqb):
                    for (l_ap, l_res, r_ap, r_res) in blk_terms(jk, i):
                        tl.append((l_ap, l_res, r_ap, r_res, base + (i - i0) * 128, 128))
                for ti, (l_ap, l_res, r_ap, r_res, c0, w) in enumerate(tl):
                    MM(STp[:, c0:c0 + w], l_ap, r_ap, ti == 0, ti == len(tl) - 1, l_res + r_res, [STr])
                spans.append((jk, base + off, wd, off))
            lo = spans[0][1]
            hi = spans[-1][1] + spans[-1][2]
            if len(spans) == 1 or all(sp[3] == 0 for sp in spans):
                ACTV(pt[:, lo:hi], STp[:, lo:hi], AF.Exp, [STr], [ptr])
            else:
                for (jk, c0, w, off) in spans:
                    ACTV(pt[:, c0:c0 + w], STp[:, c0:c0 + w], AF.Exp, [STr], [ptr])
            return pt, ptr, spans

        def stage_pv(pt, ptr, spans):
            for (jk, c0, w, off) in spans:
                v_ap, v_res = v_of_j(jk)
                if rs_in_ot:
                    MM(OTp[:, off:NQ], v_ap, pt[:, c0:c0 + w], jk == 0, False, v_res + [ptr], [OTr], skip=True)
                    MM(RSp[:, off:NQ], onesb[:], pt[:, c0:c0 + w], False, jk == jlast, [onesb_r, ptr], [RSr], skip=True)
                else:
                    MM(OTp[:, off:NQ], v_ap, pt[:, c0:c0 + w], jk == 0, jk == jlast, v_res + [ptr], [OTr])
                    MM(RSp[:, off:NQ], onesb[:], pt[:, c0:c0 + w], jk == 0, jk == jlast, [onesb_r, ptr], [RSr])

        pend = stage_qk(*chunks[0])
        for ci in range(1, len(chunks)):
            nxt = stage_qk(*chunks[ci])
            stage_pv(*pend)
            pend = nxt
        stage_pv(*pend)
        rs_ap, rs_res = rs_bufs[at_rr[0] % len(rs_bufs)]
        ACTV(rs_ap[:, 0:NQ], RSp[:, 0:NQ], AF.Ln, [RSr], rs_res)
        ACTV(rs_ap[:, 0:NQ], rs_ap[:, 0:NQ], AF.Exp, rs_res, rs_res, scale=-1.0)
        TT('dve', out_ap, OTp[:, 0:NQ], rs_ap[:, 0:NQ], ALU.mult, ([OTr] if rs_in_ot else [OTr]) + rs_res, out_res)

    def guo(l, br, OX, slots5, mpairs, tmp_pg):
        goff = {'A': OFF['ga'], 'B': OFF['gb'], 'C': OFF['gc']}[br]
        wup, wup_r = wload(slots5[0], 4, 1024, [(0, 1024, w_up_d[br][l, :, :])])
        wga, wga_r = wload(slots5[1], 8, 512, [(0, 512, w_in_d[l, :, goff:goff + 512])])
        wgb, wgb_r = wload(slots5[2], 8, 512, [(0, 512, w_in_d[l, :, goff + 512:goff + 1024])])
        woa, woa_r = wload(slots5[3], 8, 512, [(0, 512, w_out_d[l, :, 0:512])])
        wob, wob_r = wload(slots5[4], 8, 512, [(0, 512, w_out_d[l, :, 512:1024])])
        for g in range(NG):
            mp = mpairs[g % len(mpairs)]
            for n in range(NCH):
                wg_, wg_r_ = (wga, wga_r) if n < 4 else (wgb, wgb_r)
                nn = n % 4
                pg_, pgr = psum('mm', [0, 1])
                for c in range(NCH):
                    MM(pg_, wg_[:, c, nn * 128:(nn + 1) * 128], HT[:, c, g * 512:(g + 1) * 512], c == 0, c == NCH - 1,
                       wg_r_ + [HTr[c][g]], [pgr])
                pu_, pur = psum('st', [2, 3])
                for c in range(4):
                    MM(pu_, wup[:, c, n * 128:(n + 1) * 128], OX[c].b[:, g * 512:(g + 1) * 512], c == 0, c == 3,
                       wup_r + [OX[c].q[g]], [pur])
                tq = n % 4
                ACTV(tmp_pg.b[:, tq * 512:(tq + 1) * 512], pg_, AF.Sigmoid, [pgr], [tmp_pg.q[tq]])
                mpg = mp[n // 4]
                TT('dve', mpg.b[:, (n % 4) * 512:(n % 4) * 512 + 512], tmp_pg.b[:, tq * 512:(tq + 1) * 512], pu_, ALU.mult,
                   [tmp_pg.q[tq], pur], [mpg.q[n % 4]])
            for n2 in range(NCH):
                wo_, wo_r_ = (woa, woa_r) if n2 < 4 else (wob, wob_r)
                nn = n2 % 4
                py, pyr = psum('gy', [4, 5, 6, 7])
                for c in range(NCH):
                    mpg = mp[c // 4]
                    MM(py, wo_[:, c, nn * 128:(nn + 1) * 128], mpg.b[:, (c % 4) * 512:(c % 4) * 512 + 512], c == 0,
                       c == NCH - 1, wo_r_ + [mpg.q[c % 4]], [pyr])
                TT('dve', XT[:, n2, g * 512:(g + 1) * 512], XT[:, n2, g * 512:(g + 1) * 512], py, ALU.add,
                   [XTr[n2][g], pyr], [XTr[n2][g]])

    def branch_C(l):
        KVN = [pages[0], pages[1]]
        KNT = [pages[2 + h] for h in range(4)]
        VCv = GP[:, 6:10, :].rearrange("p a b -> p (a b)").bitcast(BF16).rearrange("p (i h d) -> p i h d", i=16, h=4)
        VC_r = [pages[6 + a].all for a in range(4)]
        KPET = pages[10]
        QN = [pages[11], pages[12], pages[13]]
        OC = [pages[14 + h] for h in range(4)]
        SM = pages[18]
        RAW = pages[19]
        W1, W1_r = wload(20, 8, 384, [(0, 256, w_in_d[l, :, OFF['ckv']:OFF['ckv'] + 256]),
                                      (256, 64, w_in_d[l, :, OFF['ckpe']:OFF['ckpe'] + 64]),
                                      (320, 32, w_in_d[l, :, OFF['ckpe'] + 32:OFF['ckpe'] + 64]),
                                      (352, 32, w_in_d[l, :, OFF['ckpe']:OFF['ckpe'] + 32])])
        W2, W2_r = wload(22, 2, 1024, [(0, 1024, w_kvb_d[l, :, :])])
        W2v = W2.rearrange("p k (h two d) -> p k h two d", h=4, two=2)
        for g in range(NG):
            gs = slice(g * 512, (g + 1) * 512)
            raws = []
            for c2 in range(2):
                pp, ppr = psum('mmw', [0, 1, 2, 3, 4, 5])
                for c in range(NCH):
                    MM(pp, W1[:, c, c2 * 128:(c2 + 1) * 128], HT[:, c, gs], c == 0, c == NCH - 1, W1_r + [HTr[c][g]], [ppr])
                rap = RAW.f[:, c2 * 512:(c2 + 1) * 512]
                rres = [RAW.q[2 * c2], RAW.q[2 * c2 + 1]]
                CP('act', rap, pp, [ppr], rres)
                raws.append((rap, rres))
            rbp = pages[11]
            rb = rbp.f[:, 0:512]
            rb_res = [rbp.q[0], rbp.q[1]]
            rbc_from(lambda c: raws[c][0], lambda c: raws[c][1], 2, 1.0 / 256, g, [pages[12]], rb, rb_res)
            for c2 in range(2):
                STT(KVN[c2].b[:, gs], raws[c2][0], gvec[:, VC_KVN(l) + c2:VC_KVN(l) + c2 + 1], rb, ALU.mult, ALU.mult,
                    raws[c2][1] + [gvec_r] + rb_res, [KVN[c2].q[g]])
            pa, par = psum('mmw', [0, 1, 2, 3, 4, 5])
            for c in range(NCH):
                MM(pa[0:64, :], W1[:, c, 256:320], HT[:, c, gs], c == 0, c == NCH - 1, W1_r + [HTr[c][g]], [par])
            pb, pbr = psum('mmw', [0, 1, 2, 3, 4, 5])
            for c in range(NCH):
                MM(pb[0:64, :], W1[:, c, 320:384], HT[:, c, gs], c == 0, c == NCH - 1, W1_r + [HTr[c][g]], [pbr])
            t1 = pages[13].f[0:64, 0:512]
            t1r = [pages[13].q[0], pages[13].q[1]]
            t2 = pages[13].f[0:64, 512:1024]
            t2r = [pages[13].q[2], pages[13].q[3]]
            TT('dve', t1, pa[0:64, :], cosT[:, gs], ALU.mult, [par, cosT_r], t1r)
            TT('dve', t2, pb[0:64, :], sinT[:, gs], ALU.mult, [pbr, sinT_r], t2r)
            TT('dve', KPET.b[0:64, gs], t1, t2, ALU.add, t1r + t2r, [KPET.q[g]])
            for h in range(4):
                pp, ppr = psum('mmw', [0, 1, 2, 3, 4, 5])
                for c2 in range(2):
                    MM(pp, W2v[:, c2, h, 0, :], KVN[c2].b[:, gs], c2 == 0, c2 == 1, W2_r + [KVN[c2].q[g]], [ppr])
                CP('act', KNT[h].b[:, gs], pp, [ppr], [KNT[h].q[g]])
            for ti in range(4):
                i = g * 4 + ti
                pp, ppr = psum('mmw', [0, 1, 2, 3, 4, 5])
                for c2 in range(2):
                    MM(pp.rearrange("p (h d) -> p h d", h=4), KVN[c2].b[:, i * 128:(i + 1) * 128], W2v[:, c2, :, 1, :],
                       c2 == 0, c2 == 1, W2_r + [KVN[c2].q[g]], [ppr])
                CP('dve', VCv[:, i, :, :], pp.rearrange("p (h d) -> p h d", h=4), [ppr], VC_r[i // 4])
        dump('kpeT', KPET.b[0:64, :], KPET.all)
        dump('knT0', KNT[0].b, KNT[0].all)
        W3, W3_r = wload(20, 8, 384, [(0, 384, w_in_d[l, :, OFF['cq']:OFF['cq'] + 384])])
        RAW2 = pages[18]
        for g in range(NG):
            gs = slice(g * 512, (g + 1) * 512)
            raws = []
            for c3 in range(3):
                pp, ppr = psum('mmw', [0, 1, 2, 3, 4, 5])
                for c in range(NCH):
                    MM(pp, W3[:, c, c3 * 128:(c3 + 1) * 128], HT[:, c, gs], c == 0, c == NCH - 1, W3_r + [HTr[c][g]], [ppr])
                pgx = RAW if c3 < 2 else RAW2
                cc = c3 % 2
                rap = pgx.f[:, cc * 512:(cc + 1) * 512]
                rres = [pgx.q[2 * cc], pgx.q[2 * cc + 1]]
                CP('act', rap, pp, [ppr], rres)
                raws.append((rap, rres))
            rbp = pages[0]
            rb = rbp.f[:, 0:512]
            rb_res = [rbp.q[0], rbp.q[1]]
            rbc_from(lambda c: raws[c][0], lambda c: raws[c][1], 3, 1.0 / 384, g, [pages[1]], rb, rb_res)
            for c3 in range(3):
                STT(QN[c3].b[:, gs], raws[c3][0], gvec[:, VC_QN(l) + c3:VC_QN(l) + c3 + 1], rb, ALU.mult, ALU.mult,
                    raws[c3][1] + [gvec_r] + rb_res, [QN[c3].q[g]])
        qb = w_qb_d[l]
        W4, W4_r = wload(20, 3, 512, [(h * 128, 128, qb[:, h * 192:h * 192 + 128]) for h in range(4)])
        p5 = []
        for h in range(4):
            p5.append((h * 64, 64, qb[:, h * 192 + 128:h * 192 + 192]))
            p5.append((256 + h * 64, 32, qb[:, h * 192 + 160:h * 192 + 192]))
            p5.append((256 + h * 64 + 32, 32, qb[:, h * 192 + 128:h * 192 + 160]))
        W5, W5_r = wload(22, 3, 512, p5)
        QNG = pages[18]
        QPG = pages[0]
        QnTg = QNG.b.rearrange("p (h t) -> p h t", h=4)
        QpeTg = QPG.b[0:64, :].rearrange("p (h t) -> p h t", h=4)
        PTP = pages[1]
        pt_bufs = [(PTP.b[:, 0:512], PTP.q[0]), (PTP.b[:, 512:1024], PTP.q[1])]
        rs_bufs = [(PTP.f[:, 512:1024], [PTP.q[2], PTP.q[3]])]
        for G in range(4):
            gs = slice(G * 512, (G + 1) * 512)
            for h in range(4):
                pp, ppr = psum('mm', [0, 1])
                for c3 in range(3):
                    MM(pp, W4[:, c3, h * 128:(h + 1) * 128], QN[c3].b[:, gs], c3 == 0, c3 == 2, W4_r + [QN[c3].q[G]], [ppr])
                ACTV(QnTg[:, h, :], pp, AF.Copy, [ppr], [QNG.q[h]], scale=C_SCALE)
                pa, par = psum('mm', [0, 1])
                for c3 in range(3):
                    MM(pa[0:64, :], W5[:, c3, h * 64:(h + 1) * 64], QN[c3].b[:, gs], c3 == 0, c3 == 2, W5_r + [QN[c3].q[G]], [par])
                pb, pbr = psum('mm', [0, 1])
                for c3 in range(3):
                    MM(pb[0:64, :], W5[:, c3, 256 + h * 64:256 + (h + 1) * 64], QN[c3].b[:, gs], c3 == 0, c3 == 2,
                       W5_r + [QN[c3].q[G]], [pbr])
                t1 = RAW.f[0:64, 0:512]
                t1r = [RAW.q[0], RAW.q[1]]
                t2 = RAW.f[0:64, 512:1024]
                t2r = [RAW.q[2], RAW.q[3]]
                STT(t1, pa[0:64, :], C_SCALE, cosT[:, gs], ALU.mult, ALU.mult, [par, cosT_r], t1r)
                STT(t2, pb[0:64, :], C_SCALE, sinT[:, gs], ALU.mult, ALU.mult, [pbr, sinT_r], t2r)
                TT('dve', QpeTg[:, h, :], t1, t2, ALU.add, t1r + t2r, [QPG.q[h]])
            for h in range(4):
                def wide(j, h=h):
                    ks = slice(j * 128, (j + 1) * 128)
                    return [(KNT[h].b[:, ks], [KNT[h].q[j // 4]], QnTg[:, h, :], [QNG.q[h]]),
                            (KPET.b[0:64, ks], [KPET.q[j // 4]], QpeTg[:, h, :], [QPG.q[h]])]

                def blk(j, i):
                    if j == i:
                        return [(cmaskb[:], [cmaskb_r], identb[:], [identb_r])]
                    return []
                attn_group(G * 4, 4, wide, blk, lambda j, h=h: (VCv[:, j, h, :], VC_r[j // 4]),
                           OC[h].b[:, gs], [OC[h].q[G]], pt_bufs, rs_bufs)
        for h in range(4):
            dump(f'ocT{h}', OC[h].b, OC[h].all)
        guo(l, 'C', OC, [20, 22, 0, 2, 4], [(pages[6], pages[7]), (pages[8], pages[9])], pages[10])
    def branch_A(l):
        KT = pages[0]
        VTv = pages[1].b.rearrange("p (i d) -> p i d", i=16)
        IKT = pages[2]
        SCv = GP[:, 3:5, :].rearrange("p a b -> p (a b)")
        SC_r = pages[3].all + pages[4].all
        OA = [pages[6 + h] for h in range(4)]
        SM = pages[10]
        RH = pages[11]
        W_aq, W_aq_r = wload(12, 8, 512, [(0, 512, w_in_d[l, :, OFF['aq']:OFF['aq'] + 512])])
        W_iq, W_iq_r = wload(14, 8, 512, [(0, 512, w_in_d[l, :, OFF['iq']:OFF['iq'] + 512])])
        W_kv, W_kv_r = wload(16, 8, 256, [(0, 256, w_in_d[l, :, OFF['ak']:OFF['ak'] + 256])])
        W_ix, W_ix_r = wload(18, 8, 136, [(0, 64, w_in_d[l, :, OFF['ik']:OFF['ik'] + 64]),
                                          (64, 64, w_in_d[l, :, OFF['ik']:OFF['ik'] + 64]),
                                          (128, 8, w_in_d[l, :, OFF['iw']:OFF['iw'] + 8])])
        for g in range(NG):
            gs = slice(g * 512, (g + 1) * 512)
            pp, ppr = psum('mmw', [0, 1, 2, 3, 4, 5])
            for c in range(NCH):
                MM(pp, W_kv[:, c, 0:128], HT[:, c, gs], c == 0, c == NCH - 1, W_kv_r + [HTr[c][g]], [ppr])
            CP('act', KT.b[:, gs], pp, [ppr], [KT.q[g]])
            pp, ppr = psum('mmw', [0, 1, 2, 3, 4, 5])
            for c in range(NCH):
                MM(pp, W_ix[:, c, 0:128], HT[:, c, gs], c == 0, c == NCH - 1, W_ix_r + [HTr[c][g]], [ppr])
            CP('dve', IKT.b[:, gs], pp, [ppr], [IKT.q[g]])
            pp, ppr = psum('mmw', [0, 1, 2, 3, 4, 5])
            for ti in range(4):
                i = g * 4 + ti
                for c in range(NCH):
                    MM(pp[:, ti * 128:(ti + 1) * 128], HT[:, c, i * 128:(i + 1) * 128], W_kv[:, c, 128:256], c == 0,
                       c == NCH - 1, W_kv_r + [HTr[c][g]], [ppr])
            CP('act', VTv[:, g * 4:(g + 1) * 4, :], pp.rearrange("p (i d) -> p i d", i=4), [ppr], [pages[1].q[g]])
            pp, ppr = psum('mmw', [0, 1, 2, 3, 4, 5])
            for ti in range(4):
                i = g * 4 + ti
                for c in range(NCH):
                    MM(pp[:, ti * 8:(ti + 1) * 8], HT[:, c, i * 128:(i + 1) * 128], W_ix[:, c, 128:136], c == 0,
                       c == NCH - 1, W_ix_r + [HTr[c][g]], [ppr])
            CP('dve', IW[:, g * 4:(g + 1) * 4, :], pp[:, 0:32].rearrange("p (i d) -> p i d", i=4), [ppr], [IW_r])
        QTi = SM.b[:, 0:512].rearrange("p (h t) -> p h t", h=4)
        IQTi = SM.b[:, 512:1024].rearrange("p (h t) -> p h t", h=4)
        pt_bufs = [(SM.b[:, 1024:1536], SM.q[2]), (SM.b[:, 1536:2048], SM.q[3])]
        SCs = [(SCv, SC_r),
               (GP[:, 16:18, :].rearrange("p a b -> p (a b)"), pages[16].all + pages[17].all)]
        MBs = [pages[5], pages[21], pages[18], pages[19]]
        IQs = [(SM.b[:, 512:1024].rearrange("p (h t) -> p h t", h=4), SM.q[1]),
               (pages[22].b[:, 0:512].rearrange("p (h t) -> p h t", h=4), pages[22].q[0])]
        RSP = pages[20]
        rs_bufs = [(RSP.f[:, 0:256], [RSP.q[0]]), (RSP.f[:, 256:512], [RSP.q[1]])]
        BW = 32
        bR = [dict(bnd=Res(f"bis{sl}.bnd"), mid=Res(f"bis{sl}.mid"), cnt=Res(f"bis{sl}.cnt"), tmp=Res(f"bis{sl}.tmp"),
                   stp=Res(f"bis{sl}.stp"), thr=Res(f"bis{sl}.thr")) for sl in range(2)]

        def stage2(i, MB):
            g = i // 4
            ts_ = slice(i * 128, (i + 1) * 128)
            pp, ppr = psum('mm', [0, 1])
            for h in range(4):
                for c in range(NCH):
                    MM(pp[:, h * 128:(h + 1) * 128], W_aq[:, c, h * 128:(h + 1) * 128], HT[:, c, ts_], c == 0, c == NCH - 1,
                       W_aq_r + [HTr[c][g]], [ppr])
            ACTV(QTi, pp.rearrange("p (h t) -> p h t", h=4), AF.Copy, [ppr], [SM.q[0]], scale=A_SCALE)
            yield
            for h in range(4):
                def wide(j, h=h):
                    ks = slice(j * 128, (j + 1) * 128)
                    return [(KT.b[:, ks], [KT.q[j // 4]], QTi[:, h, :], [SM.q[0]])]

                def blk(j, i_, h=h, MB=MB):
                    ks = slice(j * 128, (j + 1) * 128)
                    tl = [(MB.b[:, ks], [MB.q[j // 4]], identb[:], [identb_r])]
                    if j == i_:
                        tl.append((biasT[:, h * 2, :], [biasT_r], identb[:], [identb_r]))
                    elif j == i_ - 1:
                        tl.append((biasT[:, h * 2 + 1, :], [biasT_r], identb[:], [identb_r]))
                    return tl
                attn_group(i, 1, wide, blk, lambda j: (VTv[:, j, :], [pages[1].q[j // 4]]), OA[h].b[:, ts_], [OA[h].q[g]],
                           pt_bufs, rs_bufs, rs_in_ot=True)
                yield

        RH2 = pages[23]
        RBs = [(RH.f[:, 0:512], [RH.q[0], RH.q[1]]), (RH.f[:, 512:1024], [RH.q[2], RH.q[3]]),
               (RH2.f[:, 0:512], [RH2.q[0], RH2.q[1]]), (RH2.f[:, 512:1024], [RH2.q[2], RH2.q[3]])]
        rb_rr = [0]

        def scores(i, slot, other):
            g = i // 4
            ts_ = slice(i * 128, (i + 1) * 128)
            W = (i + 1) * 128
            nq = (W + 255) // 256
            SCv_, SC_r_ = SCs[slot]
            IQTi_, iq_r = IQs[slot]
            b0 = slot * BW
            pp, ppr = psum('mm', [0, 1])
            for j in range(4):
                for c in range(NCH):
                    MM(pp[:, j * 128:(j + 1) * 128], W_iq[:, c, j * 128:(j + 1) * 128], HT[:, c, ts_], c == 0, c == NCH - 1,
                       W_iq_r + [HTr[c][g]], [ppr])
            CP('dve', IQTi_, pp.rearrange("p (h t) -> p h t", h=4), [ppr], [iq_r])
            for hh in range(8):
                po = (hh % 2) * 64
                for k0 in range(0, W, 512):
                    kw = min(512, W - k0)
                    lp, lpr = psum('lg', [6, 7, 0, 1])
                    MM(lp[:, 0:kw], IQTi_[po:po + 64, hh // 2, :], IKT.b[po:po + 64, k0:k0 + kw], True, True,
                       [iq_r] + [IKT.q[kq] for kq in range(k0 // 512, (k0 + kw + 511) // 512)], [lpr])
                    rhf, rhr = RBs[rb_rr[0] % 4]
                    rb_rr[0] += 1
                    rh = rhf[:, 0:kw]
                    ACTV(rh, lp[:, 0:kw], AF.Relu, [lpr], rhr)
                    scr = SC_r_[k0 // 256:(k0 + kw + 255) // 256]
                    if hh == 0:
                        TS('dve', SCv_[:, k0:k0 + kw], rh, IW[:, i, 0:1], None, ALU.mult, None, rhr + [IW_r], scr)
                    else:
                        STT(SCv_[:, k0:k0 + kw], rh, IW[:, i, hh:hh + 1], SCv_[:, k0:k0 + kw], ALU.mult, ALU.add,
                            rhr + [IW_r] + scr, scr)
                if other is not None and hh % 4 == 3:
                    next(other, None)
            scw = SC_r_[0:nq]
            if i >= 2:
                k.op('dve', lambda E: E.tensor_reduce(out=bis[:, b0:b0 + 1], in_=SCv_[:, 0:W], axis=AX.X, op=ALU.max),
                     reads=scw, writes=[bR[slot]['bnd']])
                k.op('dve', lambda E: E.tensor_reduce(out=bis[:, b0 + 1:b0 + 2], in_=SCv_[:, 0:W], axis=AX.X, op=ALU.min),
                     reads=scw, writes=[bR[slot]['bnd']])
            TT('dve', SCv_[:, i * 128:W], SCv_[:, i * 128:W], cmaskf[:], ALU.add, [cmaskf_r] + SC_r_[(i * 128) // 256:nq],
               SC_r_[(i * 128) // 256:nq])

        def bis_init(i, slot):
            b0 = slot * BW
            R = bR[slot]
            sg = 1.0 if slot == 0 else -1.0
            TT('dve', bis[:, b0 + 2:b0 + 3], bis[:, b0:b0 + 1], bis[:, b0 + 1:b0 + 2], ALU.subtract, [R['bnd']], [R['tmp']])
            TS('dve', bis[:, b0 + 8:b0 + 8 + NBIS], pow2[:], bis[:, b0 + 2:b0 + 3], sg, ALU.mult, ALU.mult,
               [pow2_r, R['tmp']], [R['stp']])
            STT(bis[:, b0 + 3:b0 + 4], bis[:, b0 + 1:b0 + 2], -sg, bis[:, b0 + 8:b0 + 9], ALU.mult, ALU.subtract,
                [R['bnd'], R['stp']], [R['mid']])

        def bis_pass(i, slot, MB):
            W = (i + 1) * 128
            nq = (W + 255) // 256
            SCv_, SC_r_ = SCs[slot]
            b0 = slot * BW
            R = bR[slot]
            if slot == 0:
                ACTV(MB.b[:, 0:W], SCv_[:, 0:W], AF.Sign, SC_r_[0:nq] + [R['mid']], MB.all + [R['cnt']],
                     bias=bis[:, b0 + 3:b0 + 4], accum_out=bis[:, b0 + 4:b0 + 5])
            else:
                TS('dve', MB.b[:, 0:W], SCv_[:, 0:W], bis[:, b0 + 3:b0 + 4], 0.0, ALU.is_gt, ALU.add,
                   SC_r_[0:nq] + [R['mid']], MB.all + [R['cnt']], accum_out=bis[:, b0 + 4:b0 + 5])

        def bis_update(i, slot, it):
            W = (i + 1) * 128
            b0 = slot * BW
            R = bR[slot]
            thr_cnt = float(512 - W) if slot == 0 else 256.0
            if it < NBIS - 1:
                TS('dve', bis[:, b0 + 5:b0 + 6], bis[:, b0 + 4:b0 + 5], thr_cnt, 0.5, ALU.is_lt, ALU.subtract,
                   [R['cnt']], [R['tmp']])
                STT(bis[:, b0 + 3:b0 + 4], bis[:, b0 + 5:b0 + 6], bis[:, b0 + 8 + it:b0 + 9 + it], bis[:, b0 + 3:b0 + 4],
                    ALU.mult, ALU.add, [R['tmp'], R['stp'], R['mid']], [R['mid']])
            else:
                TS('dve', bis[:, b0 + 5:b0 + 6], bis[:, b0 + 4:b0 + 5], thr_cnt, None, ALU.is_lt, None, [R['cnt']], [R['tmp']])
                if slot == 0:
                    STT(bis[:, b0 + 6:b0 + 7], bis[:, b0 + 5:b0 + 6], bis[:, b0 + 8 + it:b0 + 9 + it], bis[:, b0 + 3:b0 + 4],
                        ALU.mult, ALU.add, [R['tmp'], R['stp'], R['mid']], [R['tmp']])
                    TS('dve', bis[:, b0 + 7:b0 + 8], bis[:, b0 + 6:b0 + 7], -1.0, None, ALU.mult, None, [R['tmp']], [R['thr']])
                else:
                    STT(bis[:, b0 + 7:b0 + 8], bis[:, b0 + 5:b0 + 6], bis[:, b0 + 8 + it:b0 + 9 + it], bis[:, b0 + 3:b0 + 4],
                        ALU.mult, ALU.add, [R['tmp'], R['stp'], R['mid']], [R['thr']])

        def mask(i, slot, MB):
            W = (i + 1) * 128
            nq = (W + 255) // 256
            SCv_, SC_r_ = SCs[slot]
            b0 = slot * BW
            TS('dve', MB.b[:, 0:W], SCv_[:, 0:W], bis[:, b0 + 7:b0 + 8], NEGM, ALU.is_lt, ALU.mult,
               SC_r_[0:nq] + [bR[slot]['thr']], MB.q[0:(W + 511) // 512])

        def chain(gens):
            for g_ in gens:
                if g_ is not None:
                    yield from g_

        prev = None
        for p in range(8):
            ia, ib = 2 * p, 2 * p + 1
            MBa, MBb = MBs[(p % 2) * 2], MBs[(p % 2) * 2 + 1]
            scores(ia, 1, prev)
            scores(ib, 0, prev)
            if p >= 1:
                bis_init(ia, 1)
                bis_init(ib, 0)
                for it in range(NBIS):
                    bis_pass(ib, 0, MBb)
                    bis_pass(ia, 1, MBa)
                    bis_update(ib, 0, it)
                    bis_update(ia, 1, it)
                    if prev is not None:
                        next(prev, None)
            else:
                MEMSET('dve', bis[:, 7:8], -1e29, [bR[0]['thr']])
                MEMSET('dve', bis[:, BW + 7:BW + 8], -1e29, [bR[1]['thr']])
            mask(ia, 1, MBa)
            mask(ib, 0, MBb)
            if prev is not None:
                for _ in prev:
                    pass
            prev = chain([stage2(ia, MBa), stage2(ib, MBb)])
        for _ in prev:
            pass
        for h in range(4):
            dump(f'oaT{h}', OA[h].b, OA[h].all)
        guo(l, 'A', OA, [12, 14, 16, 18, 20], [(pages[0], pages[1]), (pages[2], pages[3])], pages[4])
    def branch_B(l):
        QTL, KTL, KHL, KHT, VTp, GT, OBraw, SM, RBp = (pages[7], pages[8], pages[9], pages[10], pages[11], pages[16],
                                                       pages[17], pages[18], pages[19])
        OB = [pages[12 + h] for h in range(4)]
        KHTv = KHT.b.rearrange("p (i d) -> p i d", i=16)
        VTv = VTp.b.rearrange("p (i d) -> p i d", i=16)

        resetm = SM.b[:, 1536:2048]
        resetm_r = SM.q[3]
        MEMSET('dve', resetm, 1.0, [resetm_r])
        MEMSET('dve', resetm.rearrange("p (c t) -> p c t", t=64)[:, :, 0:1], 0.0, [resetm_r])

        def half(pg, g):
            hf = g % 2
            return pg.f[:, hf * 512:(hf + 1) * 512], [pg.q[2 * hf], pg.q[2 * hf + 1]]

        for h in range(4):
            col = l * 4 + h
            o = OFF
            W, W_r = wload(20 + 2 * ((h + 1) % 2), 8, 512,
                           [(0, 128, w_in_d[l, :, o['bq'] + h * 128:o['bq'] + (h + 1) * 128]),
                            (128, 128, w_in_d[l, :, o['bf'] + h * 128:o['bf'] + (h + 1) * 128]),
                            (256, 128, w_in_d[l, :, o['bi'] + h * 128:o['bi'] + (h + 1) * 128]),
                            (384, 128, w_in_d[l, :, o['bg'] + h * 128:o['bg'] + (h + 1) * 128])])
            for g in range(NG):
                gs = slice(g * 512, (g + 1) * 512)
                E, E_r = half(pages[0], g)
                Fv, F_r = half(pages[1], g)
                Bc, B_r = half(pages[2], g)
                EB, EB_r = half(pages[3], g)
                ENB, ENB_r = half(pages[4], g)
                K32, K32_r = half(pages[5], g)
                KIN, KIN_r = half(pages[6], g)
                pf, pfr = psum('mmw', [0, 1, 2, 3, 6, 7])
                for c in range(NCH):
                    MM(pf, W[:, c, 128:256], HT[:, c, gs], c == 0, c == NCH - 1, W_r + [HTr[c][g]], [pfr])
                ACTV(E, pf, AF.Exp, [pfr], E_r, scale=-1.0)
                ACTV(E, E, AF.Ln, E_r, E_r, bias=1.0)
                ACTV(E, E, AF.Exp, E_r, E_r, scale=-1.0)
                ACTV(Fv, E, AF.Ln, E_r + [OMLB_r, LB_r], F_r, scale=OMLB[:, col:col + 1], bias=LB[:, col:col + 1])
                TS('dve', KIN, E, NOMLB[:, col:col + 1], OMLB[:, col:col + 1], ALU.mult, ALU.add,
                   E_r + [NOMLB_r, OMLB_r], KIN_r)
                k.op('dve', lambda E_, Bc=Bc, Fv=Fv: E_.tensor_tensor_scan(out=Bc, data0=resetm, data1=Fv, initial=0.0,
                                                                          op0=ALU.mult, op1=ALU.add),
                     reads=F_r + [resetm_r], writes=B_r)
                ACTV(DL[:, g * 8:(g + 1) * 8], Bc.rearrange("p (c t) -> p c t", t=64)[:, :, 63], AF.Exp, B_r, [DL_r])
                ACTV(EB, Bc, AF.Exp, B_r, EB_r)
                ACTV(ENB, Bc, AF.Exp, B_r, ENB_r, scale=-1.0)
                pq, pqr = psum('mmw', [0, 1, 2, 3, 6, 7])
                for c in range(NCH):
                    MM(pq, W[:, c, 0:128], HT[:, c, gs], c == 0, c == NCH - 1, W_r + [HTr[c][g]], [pqr])
                STT(QTL.b[:, gs], pq, B_SCALE, EB, ALU.mult, ALU.mult, [pqr] + EB_r, [QTL.q[g]])
                TT('dve', K32, KIN, ENB, ALU.mult, KIN_r + ENB_r, K32_r)
                CP('act', KTL.b[:, gs], K32, K32_r, [KTL.q[g]])
                TT('dve', KHL.b[:, gs].rearrange("p (c t) -> p c t", t=64), K32.rearrange("p (c t) -> p c t", t=64),
                   DL[:, g * 8:(g + 1) * 8].unsqueeze(2).to_broadcast([128, 8, 64]), ALU.mult, K32_r + [DL_r], [KHL.q[g]])
                pv, pvr = psum('mmw', [0, 1, 2, 3, 6, 7])
                for ti in range(4):
                    i = g * 4 + ti
                    for c in range(NCH):
                        MM(pv[:, ti * 128:(ti + 1) * 128], HT[:, c, i * 128:(i + 1) * 128], W[:, c, 256:384], c == 0,
                           c == NCH - 1, W_r + [HTr[c][g]], [pvr])
                CP('act', VTv[:, g * 4:(g + 1) * 4, :], pv.rearrange("p (i d) -> p i d", i=4), [pvr], [VTp.q[g]])
                pt_, ptr_ = psum('mmw', [0, 1, 2, 3, 6, 7])
                ptb = pt_.bitcast(BF16)
                for ti in range(4):
                    i = g * 4 + ti
                    TR(ptb[:, ti * 128:(ti + 1) * 128], KHL.b[:, i * 128:(i + 1) * 128], identb[:], [KHL.q[g], identb_r], [ptr_])
                CP('dve', KHTv[:, g * 4:(g + 1) * 4, :], ptb[:, 0:512].rearrange("p (i d) -> p i d", i=4), [ptr_], [KHT.q[g]])
            for g in range(NG):
                gs = slice(g * 512, (g + 1) * 512)
                pg_, pgr = psum('mmw', [0, 1, 2, 3, 6, 7])
                for c in range(NCH):
                    MM(pg_, W[:, c, 384:512], HT[:, c, gs], c == 0, c == NCH - 1, W_r + [HTr[c][g]], [pgr])
                ACTV(GT.b[:, gs], pg_, AF.Silu, [pgr], [GT.q[g]])
            atm_res = [Res(f'atm{j}') for j in range(4)]
            MEMSET('dve', S32s[0][0][:], 0.0, [S32s[0][1], SM.q[0]])
            MEMSET('dve', SBF[0][0][:], 0.0, [SBF[0][1]])
            OTp = OTr = None
            for c in range(32):
                i, hf, g = c // 2, c % 2, c // 8
                po = hf * 64
                cs = slice(c * 64, (c + 1) * 64)
                ap_, apr = psum('st', [2, 3])
                MM(ap_[po:po + 64, 0:64], KTL.b[:, cs], QTL.b[:, cs], True, True, [KTL.q[g], QTL.q[g]], [apr])
                up_, upr = psum('rs', [6, 7])
                MM(up_[:, 0:128], KHTv[po:po + 64, i, :], VTv[po:po + 64, i, :], True, True, [KHT.q[g], VTp.q[g]], [upr])
                ao = ((c // 2) % 2) * 64
                atm = SM.b[po:po + 64, ao:ao + 64]
                atm_r = atm_res[(c % 2) * 2 + (c // 2) % 2]
                TT('dve', atm, ap_[po:po + 64, 0:64], caus01[po:po + 64, :], ALU.mult, [apr, caus01_r, SM.q[0]], [atm_r])
                s_old, s_old_r = S32s[c % 2]
                s_new, s_new_r = S32s[(c + 1) % 2]
                STT(s_new[:], s_old[:], DL[:, c:c + 1], up_[:, 0:128], ALU.mult, ALU.add, [s_old_r, DL_r, upr], [s_new_r])
                nb_, nb_r = SBF[(c + 1) % 2]
                CP('act', nb_[:], s_new[:], [s_new_r], [nb_r])
                if c % 8 == 0:
                    OTp, OTr = psum('ot', [4, 5])
                sl = slice((c % 8) * 64, (c % 8) * 64 + 64)
                sbf, sbf_r = SBF[c % 2]
                MM(OTp[:, sl], sbf[:], QTL.b[:, cs], True, False, [sbf_r, QTL.q[g]], [OTr])
                MM(OTp[:, sl], VTv[po:po + 64, i, :], atm, False, True, [VTp.q[g], atm_r], [OTr])
                if c % 8 == 7:
                    CP('act', OBraw.b[:, g * 512:(g + 1) * 512], OTp, [OTr], [OBraw.q[g]])
            MEMSET('dve', DL[:, 0:1], 0.0, atm_res + [SM.q[0], DL_r])
            for g in range(NG):
                gs = slice(g * 512, (g + 1) * 512)
                rb, rb_res = half(RBp, g)
                pn, pnr = psum('nrm', [6, 7])
                ACTV(SM.b[:, 512:1024], OBraw.b[:, gs], AF.Square, [OBraw.q[g]], [SM.q[1]])
                MM(pn, onesb[:], SM.b[:, 512:1024], True, True, [onesb_r, SM.q[1]], [pnr])
                ACTV(rb, pn, AF.Ln, [pnr], rb_res, scale=1.0 / 128, bias=EPS)
                ACTV(rb, rb, AF.Exp, rb_res, rb_res, scale=-0.5)
                STT(SM.b[:, 1024:1536], OBraw.b[:, gs], gvec[:, VC_HGN(l):VC_HGN(l) + 1], rb, ALU.mult, ALU.mult,
                    [OBraw.q[g], gvec_r] + rb_res, [SM.q[2]])
                TT('dve', OB[h].b[:, gs], SM.b[:, 1024:1536], GT.b[:, gs], ALU.mult, [SM.q[2], GT.q[g]], [OB[h].q[g]])
        for h in range(4):
            dump(f'obT{h}', OB[h].b, OB[h].all)
        guo(l, 'B', OB, [0, 2, 4, 16, 18], [(pages[6], pages[7]), (pages[8], pages[9])], pages[10])

    def ffn(l):
        norm_to_HT(VC_FFN(l), 2, 4)
        hid = [pages[i] for i in (0, 1, 5, 6, 7, 8, 9, 10)]
        tmp = pages[11]
        slots = [12, 18, 14, 16, 20, 22]
        sr = [0]

        def nslot():
            s_ = slots[sr[0] % len(slots)]
            sr[0] += 1
            return s_
        f0 = 0
        while f0 < NFF:
            nf = min(8, NFF - f0)
            downs = []
            for s0 in range(0, nf, 4):
                ns = min(4, nf - s0)
                fa = f0 + s0
                wg, wg_r = wload(nslot(), 8, ns * 128, [(0, ns * 128, wg_d[l, :, fa * 128:(fa + ns) * 128])])
                wu, wu_r = wload(nslot(), 8, ns * 128, [(0, ns * 128, wu_d[l, :, fa * 128:(fa + ns) * 128])])
                wdn, wdn_r = wload(nslot(), ns, 1024, [(0, 1024, wd_d[l, fa * 128:(fa + ns) * 128, :])])
                downs.append((s0, ns, wdn, wdn_r))
                for j in range(ns):
                    hp = hid[s0 + j]
                    for g in range(NG):
                        gs = slice(g * 512, (g + 1) * 512)
                        pg_, pgr = psum('ffg', [0, 1])
                        for c in range(NCH):
                            MM(pg_, wg[:, c, j * 128:(j + 1) * 128], HT[:, c, gs], c == 0, c == NCH - 1, wg_r + [HTr[c][g]], [pgr])
                        pu_, pur = psum('ffu', [2, 3])
                        for c in range(NCH):
                            MM(pu_, wu[:, c, j * 128:(j + 1) * 128], HT[:, c, gs], c == 0, c == NCH - 1, wu_r + [HTr[c][g]], [pur])
                        tq = g % 4
                        ACTV(tmp.b[:, tq * 512:(tq + 1) * 512], pg_, AF.Silu, [pgr], [tmp.q[tq]])
                        TT('dve', hp.b[:, gs], tmp.b[:, tq * 512:(tq + 1) * 512], pu_, ALU.mult, [tmp.q[tq], pur], [hp.q[g]])
            for n in range(NCH):
                for g in range(NG):
                    gs = slice(g * 512, (g + 1) * 512)
                    py, pyr = psum('ffy', [4, 5, 6, 7])
                    first = True
                    for (s0, ns, wdn, wdn_r) in downs:
                        for j in range(ns):
                            hp = hid[s0 + j]
                            MM(py, wdn[:, j, n * 128:(n + 1) * 128], hp.b[:, gs], first, s0 + j == nf - 1, wdn_r + [hp.q[g]], [pyr])
                            first = False
                    TT('dve', XT[:, n, gs], XT[:, n, gs], py, ALU.add, [XTr[n][g], pyr], [XTr[n][g]])
            f0 += nf

    for l in range(layer0, layer0 + n_layers):
        if any(p in parts for p in 'ABC'):
            norm_to_HT(VC_ATTN(l), 22, 21)
        if 'A' in parts:
            branch_A(l)
        if 'B' in parts:
            branch_B(l)
        if 'C' in parts:
            branch_C(l)
        if 'ffn' in parts:
            ffn(l)
        if l == 0:
            for c in range(NCH):
                dump(f'xl0_{c}', XT[:, c, :], XTr[c])

    if 'final' in parts:
        for g in range(NG):
            rb = pages[4]
            rap = rb.f[:, 0:512]
            rres = [rb.q[0], rb.q[1]]
            rbc_from(lambda c: XT[:, c, g * 512:(g + 1) * 512], lambda c: [XTr[c][g]], NCH, 1.0 / D, g,
                     [pages[2], pages[3]], rap, rres)
            for c in range(NCH):
                STT(XT[:, c, g * 512:(g + 1) * 512], XT[:, c, g * 512:(g + 1) * 512], gvec[:, VC_FINAL + c:VC_FINAL + c + 1],
                    rap, ALU.mult, ALU.mult, [XTr[c][g], gvec_r] + rres, [XTr[c][g]])
    for i in range(16):
        g = i // 4
        og = pages[6 + (i % 2)]
        for half_ in range(2):
            pap, pr = psum('tr', [0, 1])
            for j in range(4):
                c = half_ * 4 + j
                TR(pap[:, j * 128:(j + 1) * 128], XT[:, c, i * 128:(i + 1) * 128], identf[:], [XTr[c][g], identf_r], [pr])
            CP('dve' if half_ == 0 else 'act', og.f[:, half_ * 512:(half_ + 1) * 512], pap, [pr],
               [og.q[2 * half_], og.q[2 * half_ + 1]])
        DMA('sp', out_d[i * 128:(i + 1) * 128, :], og.f, reads=og.all, is_out=True)

    k.wait_tokens('sp', k.out_tokens)
    stats = k.finish()
    return nc, es, stats


def const_inputs():
    t = np.arange(128)
    c = {}
    c["c_identf"] = np.eye(128, dtype=np.float32)
    c["c_cmaskf"] = np.where(t[None, :] <= t[:, None], 0.0, -1e30).astype(np.float32)
    c["c_cmaskb"] = np.where(t[None, :] <= t[:, None], 0.0, NEGM).astype(np.float32)
    s64 = np.arange(128) % 64
    c["c_caus01"] = (s64[:, None] <= np.arange(64)[None, :]).astype(np.float32)
    def bucket(d):
        d = np.maximum(d, 0)
        dl = np.maximum(d, 16).astype(np.float32)
        large = 16 + (np.log(dl / np.float32(16)) / np.float32(np.log(128 / 16)) * np.float32(16)).astype(np.int32)
        large = np.minimum(large, 31)
        return np.where(d < 16, d, large)
    oh = np.zeros((128, 64, 128), np.float32)
    for tbl in range(2):
        d = (t[:, None] - t[None, :]) + 128 * tbl
        bk = bucket(d)
        for b in range(32):
            oh[:, tbl * 32 + b, :] = (bk == b)
    c["c_onehot"] = oh.reshape(128, 64 * 128)
    c["c_pow2"] = np.tile((2.0 ** -(np.arange(NBIS) + 1.0)).astype(np.float32)[None, :], (128, 1))
    inv_freq = (10000.0 ** (-np.arange(0, 64, 2, dtype=np.float32) / np.float32(64))).astype(np.float32)
    rope = np.zeros((64, 2), np.float32)
    rope[:, 0] = np.concatenate([inv_freq, inv_freq])
    rope[:, 1] = np.concatenate([-np.ones(32), np.ones(32)])
    c["c_rope"] = rope
    return c


W_NAMES = ("w_in", "w_up_a", "w_up_b", "w_up_c", "w_out", "mla_w_qb", "mla_w_kvb", "w_ffn_gate", "w_ffn_up", "w_ffn_down")


def core_inputs(inputs, b, consts, vecs, relb):
    m = dict(consts)
    m["x"] = np.ascontiguousarray(inputs["x"][b])
    m["pos64"] = np.ascontiguousarray(np.broadcast_to(inputs["positions"][b][None, :], (64, T))).astype(np.int32)
    m["vecs"] = vecs
    m["relb"] = relb
    for wn in W_NAMES:
        m[wn] = inputs[wn]
    return m


def kernel(**inputs):
    inputs = {k_: np.asarray(v) for k_, v in inputs.items()}
    nc, es, stats = build(parts=('A', 'B', 'C', 'ffn', 'final'))
    consts = const_inputs()
    vecs = make_vecs(inputs)
    relb = np.ascontiguousarray(np.broadcast_to(inputs["rel_bias"].reshape(1, 128), (128, 128))).astype(np.float32)
    for wn in W_NAMES:
        inputs[wn] = np.ascontiguousarray(inputs[wn], dtype=np.float32)
    in_maps = [core_inputs(inputs, b, consts, vecs, relb) for b in range(8)]
    res = run_bass_kernel_spmd(nc, in_maps, core_ids=list(range(8)))
    es.close()
    return np.stack([np.asarray(r["out"], dtype=np.float32) for r in res.results], axis=0)
```
